# Optimizing a Trainium2 kernel written in Bass

```python
import jax, jax.numpy as jnp
from jax import lax
import numpy as np

D_MODEL = 1024
BATCH = 2
SEQ = 8192
DEPTH = 2

GRID_W = 64
CTX_LEN = 256
N_GROUPS = 4
GROUP_HEADS = 4
GROUP_WIDTH = D_MODEL // N_GROUPS
HEAD_DIM = GROUP_WIDTH // GROUP_HEADS
D_MIX = N_GROUPS * GROUP_WIDTH
A_KV_HEADS = 2
B_KV_HEADS = 2
Q_BLOCK = 128
WINDOW = 128
WIN_BLOCK = 128
MLSTM_CHUNK = 64
NA_KH = 8
NA_KW = 16
D_FF = ((8 * D_MODEL // 3 + 127) // 128) * 128
CONV_W = 3
ROPE_THETA = 10000.0
EPS = 1e-6
NEG_INF = -1e30
SPLIT_SIZES = (GROUP_WIDTH, A_KV_HEADS * HEAD_DIM, A_KV_HEADS * HEAD_DIM,
               GROUP_WIDTH, B_KV_HEADS * HEAD_DIM, B_KV_HEADS * HEAD_DIM,
               GROUP_WIDTH, GROUP_WIDTH, GROUP_WIDTH, GROUP_WIDTH, 4 * GROUP_HEADS,
               GROUP_WIDTH, GROUP_WIDTH, GROUP_WIDTH)
D_IN = sum(SPLIT_SIZES)

kernel_name = 'hybrid_parallel_heads_dit_block'


def rms_norm(x, g):
    xf = x.astype(jnp.float32)
    y = xf * lax.rsqrt(jnp.mean(xf * xf, axis=-1, keepdims=True) + EPS)
    return (y * g.astype(jnp.float32)).astype(x.dtype)


def to_heads(x, n):
    b, t, _ = x.shape
    return x.reshape(b, t, n, -1).transpose(0, 2, 1, 3)


def from_heads(x):
    b, h, t, d = x.shape
    return x.transpose(0, 2, 1, 3).reshape(b, t, h * d)


def split_proj(p):
    outs, start = [], 0
    for size in SPLIT_SIZES:
        outs.append(p[..., start:start + size])
        start += size
    return outs


def axial_angles(n_tokens):
    t = jnp.arange(n_tokens)
    row = (t // GRID_W).astype(jnp.float32)
    col = (t % GRID_W).astype(jnp.float32)
    n_freq = HEAD_DIM // 4
    freqs = ROPE_THETA ** (-jnp.arange(n_freq, dtype=jnp.float32) / n_freq)
    return row[:, None] * freqs, col[:, None] * freqs


def _rotate(x, ang):
    x1, x2 = jnp.split(x, 2, axis=-1)
    cos, sin = jnp.cos(ang).astype(x.dtype), jnp.sin(ang).astype(x.dtype)
    return jnp.concatenate([x1 * cos - x2 * sin, x1 * sin + x2 * cos], axis=-1)


def rope_2d(x, ang_r, ang_c):
    xr, xc = jnp.split(x, 2, axis=-1)
    return jnp.concatenate([_rotate(xr, ang_r), _rotate(xc, ang_c)], axis=-1)


def ctx_attend(qc, kc, vc, sink=None):
    b, hq, n, d = qc.shape
    kv = kc.shape[1]
    g = hq // kv
    qg = qc.reshape(b, kv, g, n, d)
    s = jnp.einsum('bkgqd,bksd->bkgqs', qg, kc).astype(jnp.float32) * d ** -0.5
    if sink is not None:
        sk = jnp.broadcast_to(sink.astype(jnp.float32).reshape(1, kv, g, 1, 1), (b, kv, g, n, 1))
        p = jax.nn.softmax(jnp.concatenate([s, sk], axis=-1), axis=-1)[..., :-1]
    else:
        p = jax.nn.softmax(s, axis=-1)
    o = jnp.einsum('bkgqs,bksd->bkgqd', p.astype(vc.dtype), vc)
    return o.reshape(b, hq, n, d)


def mixer_global(q, k, v, qc, kc, vc, gq, gk, ang_r, ang_c, need_ctx):
    q = rope_2d(rms_norm(q, gq), ang_r, ang_c)
    k = rope_2d(rms_norm(k, gk), ang_r, ang_c)
    qc, kc = rms_norm(qc, gq), rms_norm(kc, gk)
    b, h, t, d = q.shape
    kv = k.shape[1]
    g = h // kv
    nb = t // Q_BLOCK
    keys = jnp.concatenate([kc, k], axis=2)
    vals = jnp.concatenate([vc, v], axis=2)
    qb = q.reshape(b, kv, g, nb, Q_BLOCK, d).transpose(3, 0, 1, 2, 4, 5)

    def block(qi):
        s = jnp.einsum('bkgqd,bksd->bkgqs', qi, keys).astype(jnp.float32) * d ** -0.5
        p = jax.nn.softmax(s, axis=-1).astype(vals.dtype)
        return jnp.einsum('bkgqs,bksd->bkgqd', p, vals)

    o = lax.map(block, qb)
    o = o.transpose(1, 2, 3, 0, 4, 5).reshape(b, h, t, d)
    oc = ctx_attend(qc, kc, vc) if need_ctx else None
    return o, oc


def mixer_window(q, k, v, qc, kc, vc, sink, ang_r, ang_c, need_ctx):
    q = rope_2d(q, ang_r, ang_c)
    k = rope_2d(k, ang_r, ang_c)
    b, h, t, d = q.shape
    kv = k.shape[1]
    g = h // kv
    w = WIN_BLOCK
    nb = t // w

    def band(x):
        xb = x.reshape(b, kv, nb, w, d)
        xp = jnp.pad(xb, ((0, 0), (0, 0), (1, 1), (0, 0), (0, 0)))
        return jnp.concatenate([xp[:, :, :-2], xp[:, :, 1:-1], xp[:, :, 2:]], axis=3)

    kb, vb = band(k), band(v)
    qg = q.reshape(b, kv, g, nb, w, d)
    scale = d ** -0.5
    s_loc = jnp.einsum('bkgnqd,bknsd->bkgnqs', qg, kb).astype(jnp.float32) * scale
    q_pos = jnp.arange(nb)[:, None, None] * w + jnp.arange(w)[None, :, None]
    k_pos = jnp.arange(nb)[:, None, None] * w - w + jnp.arange(3 * w)[None, None, :]
    valid = (jnp.abs(k_pos - q_pos) <= WINDOW) & (k_pos >= 0) & (k_pos < t)
    s_loc = jnp.where(valid, s_loc, NEG_INF)
    s_ctx = jnp.einsum('bkgnqd,bkcd->bkgnqc', qg, kc).astype(jnp.float32) * scale
    sk = jnp.broadcast_to(sink.astype(jnp.float32).reshape(1, kv, g, 1, 1, 1), (b, kv, g, nb, w, 1))
    p = jax.nn.softmax(jnp.concatenate([s_loc, s_ctx, sk], axis=-1), axis=-1).astype(v.dtype)
    n_loc, n_ctx = 3 * w, kc.shape[2]
    o = (jnp.einsum('bkgnqs,bknsd->bkgnqd', p[..., :n_loc], vb)
         + jnp.einsum('bkgnqc,bkcd->bkgnqd', p[..., n_loc:n_loc + n_ctx], vc))
    o = o.reshape(b, h, t, d)
    oc = ctx_attend(qc, kc, vc, sink) if need_ctx else None
    return o, oc


def mlstm_chunkwise(q, k, v, log_i, log_f, state):
    b, h, t, d = q.shape
    nc = t // MLSTM_CHUNK
    lc = MLSTM_CHUNK

    def chunks(x):
        return jnp.moveaxis(x.reshape((b, h, nc, lc) + x.shape[3:]), 2, 0)

    causal = jnp.tril(jnp.ones((lc, lc), dtype=bool))

    def step(carry, inp):
        c_mat, n_vec, m = carry
        qb, kb, vb, li, lf = inp
        cum = jnp.cumsum(lf, axis=-1)
        logw = jnp.where(causal, cum[..., :, None] - cum[..., None, :] + li[..., None, :], NEG_INF)
        inter = cum + m[..., None]
        m_t = jnp.maximum(inter, logw.max(axis=-1))
        w_inter = jnp.exp(inter - m_t)
        s = jnp.einsum('bhtk,bhsk->bhts', qb, kb) * jnp.exp(logw - m_t[..., None])
        num = (jnp.einsum('bhts,bhsv->bhtv', s, vb)
               + w_inter[..., None] * jnp.einsum('bhvk,bhtk->bhtv', c_mat, qb))
        den = s.sum(axis=-1) + w_inter * jnp.einsum('bhk,bhtk->bht', n_vec, qb)
        hout = num / jnp.maximum(jnp.abs(den), jnp.exp(-m_t))[..., None]
        cum_end = cum[..., -1]
        log_end = cum_end[..., None] - cum + li
        m_new = jnp.maximum(cum_end + m, log_end.max(axis=-1))
        decay = jnp.exp(cum_end + m - m_new)
        w_end = jnp.exp(log_end - m_new[..., None])
        c_mat = decay[..., None, None] * c_mat + jnp.einsum('bhs,bhsv,bhsk->bhvk', w_end, vb, kb)
        n_vec = decay[..., None] * n_vec + jnp.einsum('bhs,bhsk->bhk', w_end, kb)
        return (c_mat, n_vec, m_new), hout

    state, hs = lax.scan(step, state, (chunks(q), chunks(k), chunks(v), chunks(log_i), chunks(log_f)))
    hs = jnp.moveaxis(hs, 0, 2).reshape(b, h, t, d)
    return hs, state


def mixer_mlstm(q, k, v, o, gates, qc, kc, vc, oc, gates_c, gate_bias, need_ctx):
    b, h, t, d = q.shape
    f32 = jnp.float32

    def prep_gates(g):
        g = (g + gate_bias).astype(f32)
        g = g.reshape(g.shape[0], g.shape[1], 4, h).transpose(2, 0, 3, 1)
        return g[0], jax.nn.log_sigmoid(g[1]), g[2], jax.nn.log_sigmoid(g[3])

    li_f, lf_f, li_b, lf_b = prep_gates(gates)
    lic_f, lfc_f, lic_b, lfc_b = prep_gates(gates_c)
    sc = d ** -0.5
    q32, k32, v32 = (q * sc).astype(f32), k.astype(f32), v.astype(f32)
    qc32, kc32, vc32 = (qc * sc).astype(f32), kc.astype(f32), vc.astype(f32)
    zero = (jnp.zeros((b, h, d, d), f32), jnp.zeros((b, h, d), f32), jnp.zeros((b, h), f32))

    def flip(x):
        return jnp.flip(x, axis=2)

    hc_f, st_f = mlstm_chunkwise(qc32, kc32, vc32, lic_f, lfc_f, zero)
    hc_b, st_b = mlstm_chunkwise(flip(qc32), flip(kc32), flip(vc32), flip(lic_b), flip(lfc_b), zero)
    h_f, _ = mlstm_chunkwise(q32, k32, v32, li_f, lf_f, st_f)
    h_b, _ = mlstm_chunkwise(flip(q32), flip(k32), flip(v32), flip(li_b), flip(lf_b), st_b)
    y = (jax.nn.sigmoid(o.astype(f32)) * (h_f + flip(h_b))).astype(v.dtype)
    yc = (jax.nn.sigmoid(oc.astype(f32)) * (hc_f + flip(hc_b))).astype(vc.dtype) if need_ctx else None
    return y, yc


def mixer_neighbourhood(q, k, v, qc, kc, vc, rpb, need_ctx):
    b, h, t, d = q.shape
    rows = t // GRID_W
    kh, kw = min(NA_KH, rows), NA_KW
    qg = q.reshape(b, h, rows, GRID_W, d)
    kg = k.reshape(b, h, rows, GRID_W, d)
    vg = v.reshape(b, h, rows, GRID_W, d)
    cols = jnp.arange(GRID_W)
    col_start = jnp.clip(cols - kw // 2, 0, GRID_W - kw)
    col_idx = col_start[:, None] + jnp.arange(kw)[None, :]
    dc_idx = col_idx - cols[:, None] + (NA_KW - 1)
    scale = d ** -0.5
    n_nb = kh * kw

    def row_fn(r):
        rs = jnp.clip(r - kh // 2, 0, rows - kh)
        kr = lax.dynamic_slice_in_dim(kg, rs, kh, axis=2)[:, :, :, col_idx]
        vr = lax.dynamic_slice_in_dim(vg, rs, kh, axis=2)[:, :, :, col_idx]
        qr = lax.dynamic_index_in_dim(qg, r, axis=2, keepdims=False)
        dr_idx = rs + jnp.arange(kh) - r + (NA_KH - 1)
        bias = rpb[:, dr_idx][:, :, dc_idx].transpose(0, 2, 1, 3)
        s_nb = jnp.einsum('bhwd,bhiwjd->bhwij', qr, kr).astype(jnp.float32) * scale + bias.astype(jnp.float32)
        s_c = jnp.einsum('bhwd,bhcd->bhwc', qr, kc).astype(jnp.float32) * scale
        p = jax.nn.softmax(jnp.concatenate([s_nb.reshape(b, h, GRID_W, n_nb), s_c], axis=-1), axis=-1)
        p = p.astype(v.dtype)
        p_nb = p[..., :n_nb].reshape(b, h, GRID_W, kh, kw)
        return (jnp.einsum('bhwij,bhiwjd->bhwd', p_nb, vr)
                + jnp.einsum('bhwc,bhcd->bhwd', p[..., n_nb:], vc))

    o = lax.map(row_fn, jnp.arange(rows))
    o = o.transpose(1, 2, 0, 3, 4).reshape(b, h, t, d)
    oc = ctx_attend(qc, kc, vc) if need_ctx else None
    return o, oc


def mix_out(ys, g_group, w_out):
    y = jnp.concatenate([from_heads(t) for t in ys], axis=-1)
    b, t, _ = y.shape
    y = rms_norm(y.reshape(b, t, N_GROUPS, GROUP_WIDTH), g_group.reshape(N_GROUPS, GROUP_WIDTH))
    return y.reshape(b, t, D_MIX) @ w_out


def dwconv_centred(x, w, bias):
    t = x.shape[1]
    xp = jnp.pad(x, ((0, 0), (1, 1), (0, 0)))
    return xp[:, :t] * w[0] + xp[:, 1:t + 1] * w[1] + xp[:, 2:] * w[2] + bias


def conv_ffn(h, w_up, conv_w, conv_b, w_down):
    u = dwconv_centred(h @ w_up, conv_w, conv_b)
    gate, val = jnp.split(u, 2, axis=-1)
    return (jax.nn.silu(gate) * val) @ w_down


def trunk_layer(x, ctx, mod_lat, mod_ctx, g1, g2, w_in, a_qg, a_kg, sink, c_gb, rpb, g_group,
                w_out, w_up, conv_w, conv_b, w_down, ang_r, ang_c, need_ctx):
    sh1, sc1, gt1, sh2, sc2, gt2 = jnp.split(mod_lat, 6, axis=-1)
    sh1c, sc1c, gt1c, sh2c, sc2c, gt2c = jnp.split(mod_ctx, 6, axis=-1)
    h = rms_norm(x, g1) * (1 + sc1) + sh1
    hc = rms_norm(ctx, g1) * (1 + sc1c) + sh1c
    aq, ak, av, bq, bk, bv, cq, ck, cv, co, cg, dq, dk, dv = split_proj(h @ w_in)
    aqc, akc, avc, bqc, bkc, bvc, cqc, ckc, cvc, coc, cgc, dqc, dkc, dvc = split_proj(hc @ w_in)
    nh = GROUP_HEADS
    ya, yac = mixer_global(to_heads(aq, nh), to_heads(ak, A_KV_HEADS), to_heads(av, A_KV_HEADS),
                           to_heads(aqc, nh), to_heads(akc, A_KV_HEADS), to_heads(avc, A_KV_HEADS),
                           a_qg, a_kg, ang_r, ang_c, need_ctx)
    yb, ybc = mixer_window(to_heads(bq, nh), to_heads(bk, B_KV_HEADS), to_heads(bv, B_KV_HEADS),
                           to_heads(bqc, nh), to_heads(bkc, B_KV_HEADS), to_heads(bvc, B_KV_HEADS),
                           sink, ang_r, ang_c, need_ctx)
    yc, ycc = mixer_mlstm(to_heads(cq, nh), to_heads(ck, nh), to_heads(cv, nh), to_heads(co, nh), cg,
                          to_heads(cqc, nh), to_heads(ckc, nh), to_heads(cvc, nh), to_heads(coc, nh), cgc,
                          c_gb, need_ctx)
    yd, ydc = mixer_neighbourhood(to_heads(dq, nh), to_heads(dk, nh), to_heads(dv, nh),
                                  to_heads(dqc, nh), to_heads(dkc, nh), to_heads(dvc, nh), rpb, need_ctx)
    x = x + gt1 * mix_out([ya, yb, yc, yd], g_group, w_out)
    h2 = rms_norm(x, g2) * (1 + sc2) + sh2
    x = x + gt2 * conv_ffn(h2, w_up, conv_w, conv_b, w_down)
    if need_ctx:
        ctx = ctx + gt1c * mix_out([yac, ybc, ycc, ydc], g_group, w_out)
        h2c = rms_norm(ctx, g2) * (1 + sc2c) + sh2c
        ctx = ctx + gt2c * conv_ffn(h2c, w_up, conv_w, conv_b, w_down)
    return x, ctx


def setup_inputs(seed: int = 0) -> dict:
    key = jax.random.key(seed)
    ks = jax.random.split(key, 24)
    nrm = jax.random.normal
    f32 = jnp.float32
    gate_noise = 0.1 * nrm(ks[10], (DEPTH, 4, GROUP_HEADS), f32)
    forget_bias = jnp.linspace(3.0, 6.0, GROUP_HEADS, dtype=f32)
    gate_bias = gate_noise + jnp.array([0.0, 1.0, 0.0, 1.0], f32)[None, :, None] * forget_bias[None, None, :]
    return {
        'x': nrm(ks[0], (BATCH, SEQ, D_MODEL), f32),
        'c': nrm(ks[1], (BATCH, D_MODEL), f32),
        'ctx': nrm(ks[2], (BATCH, CTX_LEN, D_MODEL), f32),
        'c_ctx': nrm(ks[3], (D_MODEL,), f32),
        'w_mod': nrm(ks[4], (DEPTH, D_MODEL, 6 * D_MODEL), f32) * (0.5 * D_MODEL ** -0.5),
        'b_mod': 0.02 * nrm(ks[5], (DEPTH, 6 * D_MODEL), f32),
        'g_norm1': 1.0 + 0.05 * nrm(ks[6], (DEPTH, D_MODEL), f32),
        'g_norm2': 1.0 + 0.05 * nrm(ks[7], (DEPTH, D_MODEL), f32),
        'w_in': nrm(ks[8], (DEPTH, D_MODEL, D_IN), f32) * D_MODEL ** -0.5,
        'a_q_gain': 1.0 + 0.05 * nrm(ks[9], (DEPTH, HEAD_DIM), f32),
        'a_k_gain': 1.0 + 0.05 * nrm(ks[11], (DEPTH, HEAD_DIM), f32),
        'b_sink': 0.5 * nrm(ks[12], (DEPTH, GROUP_HEADS), f32),
        'c_gate_bias': gate_bias.reshape(DEPTH, 4 * GROUP_HEADS),
        'd_rel_bias': 0.2 * nrm(ks[13], (DEPTH, GROUP_HEADS, 2 * NA_KH - 1, 2 * NA_KW - 1), f32),
        'g_group': 1.0 + 0.05 * nrm(ks[14], (DEPTH, D_MIX), f32),
        'w_out': nrm(ks[15], (DEPTH, D_MIX, D_MODEL), f32) * D_MIX ** -0.5,
        'w_up': nrm(ks[16], (DEPTH, D_MODEL, 2 * D_FF), f32) * D_MODEL ** -0.5,
        'conv_w': nrm(ks[17], (DEPTH, CONV_W, 2 * D_FF), f32) * CONV_W ** -0.5,
        'conv_b': 0.02 * nrm(ks[18], (DEPTH, 2 * D_FF), f32),
        'w_down': nrm(ks[19], (DEPTH, D_FF, D_MODEL), f32) * D_FF ** -0.5,
        'g_final': 1.0 + 0.05 * nrm(ks[20], (D_MODEL,), f32),
    }


def reference(x, c, ctx, c_ctx, w_mod, b_mod, g_norm1, g_norm2, w_in, a_q_gain, a_k_gain, b_sink,
              c_gate_bias, d_rel_bias, g_group, w_out, w_up, conv_w, conv_b, w_down, g_final):
    t = x.shape[1]
    ang_r, ang_c = axial_angles(t)
    for layer in range(DEPTH):
        mod_lat = (jax.nn.silu(c) @ w_mod[layer] + b_mod[layer])[:, None, :]
        mod_ctx = (jax.nn.silu(c_ctx) @ w_mod[layer] + b_mod[layer])[None, None, :]
        x, ctx = trunk_layer(x, ctx, mod_lat, mod_ctx, g_norm1[layer], g_norm2[layer], w_in[layer],
                             a_q_gain[layer], a_k_gain[layer], b_sink[layer], c_gate_bias[layer],
                             d_rel_bias[layer], g_group[layer], w_out[layer], w_up[layer],
                             conv_w[layer], conv_b[layer], w_down[layer], ang_r, ang_c,
                             layer < DEPTH - 1)
    return rms_norm(x, g_final)
```

```python
import numpy as np
import concourse.bass as bass
import concourse.mybir as mybir
from concourse.bass_utils import run_bass_kernel_spmd
from contextlib import ExitStack

F32 = mybir.dt.float32
BF16 = mybir.dt.bfloat16
I32 = mybir.dt.int32
AF = mybir.ActivationFunctionType
ALU = mybir.AluOpType
NMETA = 8


class Lin:
    def __init__(self, c=0, **co):
        self.c = c
        self.co = {n: v for n, v in co.items() if v}

    def __add__(self, o):
        if isinstance(o, int):
            return Lin(self.c + o, **self.co)
        co = dict(self.co)
        for n, v in o.co.items():
            co[n] = co.get(n, 0) + v
        return Lin(self.c + o.c, **co)

    __radd__ = __add__

    def __mul__(self, m):
        return Lin(self.c * m, **{n: v * m for n, v in self.co.items()})

    __rmul__ = __mul__

    def expr(self, V):
        e = None
        for n, v in self.co.items():
            t = V[n] * v
            e = t if e is None else e + t
        return self.c if e is None else e + self.c

    def at(self, core):
        vals = {"b": core // 4, "j": core % 4, "jh": (core % 4) // 2, "jl": (core % 4) % 2}
        return self.c + sum(v * vals[n] for n, v in self.co.items())


VB, VJ, VJH, VJL = Lin(b=1), Lin(j=1), Lin(jh=1), Lin(jl=1)


class Res:
    def __init__(self, name):
        self.name = name
        self.w = None
        self.r = []


class Buf:
    def __init__(self, t, name):
        self.t = t
        self.name = name
        self.whole = Res(name)
        self.parts = {}

    def __getitem__(self, key):
        return self.t[key]

    def p(self, key):
        return (self, key)


class KB:
    NDMA = 24

    def __init__(self, nc, stack):
        self.nc = nc
        self.root = stack
        self.stack = stack
        self.eng = {"pe": nc.tensor, "act": nc.scalar, "dve": nc.vector, "pool": nc.gpsimd, "sp": nc.sync}
        self.sem = {e: stack.enter_context(nc.semaphore("c_" + e)) for e in self.eng}
        self.cnt = {e: 0 for e in self.eng}
        self.known = {e: {} for e in self.eng}
        self.dsem = [stack.enter_context(nc.semaphore("d%d" % i)) for i in range(self.NDMA)]
        self.dval = [0] * self.NDMA
        self.dnext = 0
        self.ccsem = stack.enter_context(nc.semaphore("ccsem"))
        self.ccval = 0
        self.out_tokens = []
        self.nbuf = 0
        self.V = None

    def sb(self, shape, dtype=F32, name=None):
        self.nbuf += 1
        name = "%s_%d" % (name or "sb", self.nbuf)
        t = self.stack.enter_context(self.nc.sbuf_tensor(name, list(shape), dtype))
        return Buf(t, name)

    def ps(self, shape, dtype=F32, name=None):
        self.nbuf += 1
        name = "%s_%d" % (name or "ps", self.nbuf)
        t = self.stack.enter_context(self.nc.psum_tensor(name, list(shape), dtype))
        return Buf(t, name)

    def dram(self, name, shape, dtype, kind=None):
        if kind is None:
            t = self.nc.dram_tensor(name, list(shape), dtype)
        else:
            t = self.nc.dram_tensor(name, list(shape), dtype, kind=kind)
        return Buf(t, name)

    def _states(self, r):
        if isinstance(r, Buf):
            return [r.whole] + list(r.parts.values()), r.whole
        b, k = r
        if k not in b.parts:
            b.parts[k] = Res("%s/%s" % (b.name, k))
        return [b.whole, b.parts[k]], b.parts[k]

    def _wait(self, e, tok):
        if tok is None:
            return
        kind, a, n = tok
        if kind == "eng":
            if a == "pe" and e == "pe":
                return
            key, sem = ("eng", a), self.sem[a]
        elif kind == "cc":
            key, sem = ("cc", 0), self.ccsem
        else:
            key, sem = ("dma", a), self.dsem[a]
        if self.known[e].get(key, 0) >= n:
            return
        self.known[e][key] = n
        self.eng[e].wait_ge(sem, n)

    def _deps(self, e, reads, writes):
        for r in reads:
            for s in self._states(r)[0]:
                self._wait(e, s.w)
        for w in writes:
            for s in self._states(w)[0]:
                self._wait(e, s.w)
                for t in s.r:
                    self._wait(e, t)

    def _commit(self, tok, reads, writes):
        for r in reads:
            self._states(r)[1].r.append(tok)
        for w in writes:
            sts, own = self._states(w)
            if isinstance(w, Buf):
                for s in sts:
                    s.w = tok
                    s.r = []
            else:
                own.w = tok
                own.r = []

    def op(self, e, fn, reads=(), writes=()):
        self._deps(e, reads, writes)
        ins = fn(self.eng[e])
        self.cnt[e] += 1
        ins.then_inc(self.sem[e], 1)
        tok = ("eng", e, self.cnt[e])
        self._commit(tok, reads, writes)
        return tok

    def _dma_issue(self, q, make, reads, writes, is_output):
        self._deps(q, reads, writes)
        s = self.dnext
        self.dnext = (self.dnext + 1) % self.NDMA
        if self.dval[s] > 0:
            self._wait(q, ("dma", s, self.dval[s]))
        ins = make()
        self.dval[s] += 16
        ins.then_inc(self.dsem[s], 16)
        tok = ("dma", s, self.dval[s])
        self._commit(tok, reads, writes)
        if is_output:
            self.out_tokens.append(tok)
        return tok

    def dma(self, q, out_ap, in_ap, reads=(), writes=(), is_output=False):
        return self._dma_issue(q, lambda: self.eng[q].dma_start(out=out_ap, in_=in_ap), reads, writes, is_output)

    def load_coords(self, meta_buf):
        self._deps("pool", [meta_buf], [])
        self.rcore = self.root.enter_context(self.nc.gpsimd.register("rcore"))
        self.rbh = self.root.enter_context(self.nc.gpsimd.register("rbh"))
        self.nc.gpsimd.reg_load(self.rcore, meta_buf.t[0:1, 0:1])
        self.nc.gpsimd.reg_load(self.rbh, meta_buf.t[0:1, 1:2])

    def dma_dyn(self, out_ap, src, off, pattern, reads=(), writes=(), slow=False):
        q = "pool"
        reads = list(reads) + [src]
        self._deps(q, reads, writes)
        s = self.dnext
        self.dnext = (self.dnext + 1) % self.NDMA
        if self.dval[s] > 0:
            self._wait(q, ("dma", s, self.dval[s]))
        if set(off.co) <= {"b", "jh"}:
            reg, cores = self.rbh, [(2 * b + jh, 4 * b + 2 * jh) for b in range(2) for jh in range(2)]
        else:
            reg, cores = self.rcore, [(c, c) for c in range(8)]
        g = self.nc.gpsimd
        for v, core in cores:
            with g.If_eq(reg, v):
                g.dma_start(out=out_ap, in_=bass.AP(src.t, off.at(core), pattern), **({"allow_slow_non_contiguous": True} if slow else {})).then_inc(self.dsem[s], 16)
        g.end_ifs()
        self.dval[s] += 16
        tok = ("dma", s, self.dval[s])
        self._commit(tok, reads, writes)
        return tok

    def all_gather(self, src, dst):
        self._deps("pool", [src], [dst])
        ins = self.nc.gpsimd.collective_compute("AllGather", ALU.bypass, replica_groups=[list(range(8))],
                                                ins=[src.t[:, :]], outs=[dst.t[:, :]])
        self.ccval += 1
        ins.then_inc(self.ccsem, 1)
        tok = ("cc", 0, self.ccval)
        self._commit(tok, [src], [dst])
        return tok

    def barrier(self):
        for e in self.eng:
            for f in self.eng:
                if f != e and self.cnt[f] > 0:
                    self._wait(e, ("eng", f, self.cnt[f]))
            for s in range(self.NDMA):
                if self.dval[s] > 0:
                    self._wait(e, ("dma", s, self.dval[s]))
            if self.ccval > 0:
                self._wait(e, ("cc", 0, self.ccval))

    class _Phase:
        def __init__(self, k):
            self.k = k

        def __enter__(self):
            self.st = ExitStack()
            self.st.__enter__()
            self.prev = self.k.stack
            self.k.stack = self.st
            return self

        def __exit__(self, *a):
            if a[0] is None:
                self.k.barrier()
            self.k.stack = self.prev
            return self.st.__exit__(*a)

    def phase(self):
        return KB._Phase(self)

    def finish(self, e="sp"):
        self.barrier()
D = 1024
SEQ = 8192
NB = 2
CTX = 256
TL = 2048
TCX = 64
TC = TL + TCX
TS = CTX + SEQ
EPS = 1e-6
D_FF = 2816
NFC = D_FF // 128
NEG = -100.0
NCHK = TS // 128
NCH = TS // 64
YW = TS + 4

O_AQ, O_AK, O_AV = 0, 256, 384
O_BQ, O_BK, O_BV = 512, 768, 896
O_CQ, O_CK, O_CV, O_CO, O_CG = 1024, 1280, 1536, 1792, 2048
O_DQ, O_DK, O_DV = 2064, 2320, 2576


def _swap_idx():
    d = np.arange(64)
    i = d % 32
    return np.where(i < 16, d + 16, d - 16)


SW64 = _swap_idx()
SW128 = np.concatenate([SW64, SW64 + 64])

P_TILES = [
    ("aq0", O_AQ, "ropeA_q", ("b", 0)), ("aq1", O_AQ + 128, "ropeA_q", ("b", 1)), ("ak", O_AK, "ropeA_k", ("b", 2)),
    ("bq0", O_BQ, "ropeB_q", ("b", 3)), ("bq1", O_BQ + 128, "ropeB_q", ("b", 4)), ("bk", O_BK, "ropeB_k", ("b", 5)),
    ("dq0", O_DQ, "plain_q", ("b", 6)), ("dq1", O_DQ + 128, "plain_q", ("b", 7)),
    ("dk0", O_DK, "plain", ("b", 8)), ("dk1", O_DK + 128, "plain", ("b", 9)),
    ("av", O_AV, "tok", ("bv", 0)), ("bv", O_BV, "tok", ("bv", 1)), ("dv0", O_DV, "tok", ("bv", 2)), ("dv1", O_DV + 128, "tok", ("bv", 3)),
    ("cq0", O_CQ, "plain_q", ("c", 0)), ("cq1", O_CQ + 128, "plain_q", ("c", 1)),
    ("ck0", O_CK, "plain", ("c", 2)), ("ck1", O_CK + 128, "plain", ("c", 3)),
    ("co0", O_CO, "plain", ("c", 4)), ("co1", O_CO + 128, "plain", ("c", 5)),
    ("ck0t", O_CK, "tok", ("ct", 0)), ("ck1t", O_CK + 128, "tok", ("ct", 1)),
    ("cv0t", O_CV, "tok", ("ct", 2)), ("cv1t", O_CV + 128, "tok", ("ct", 3)),
]
B_FM = 10 * 128
B_ROWS = B_FM + 512
C_FM = 6 * 128
C_G = C_FM
C_TOK = C_FM + 16
C_ROWS = C_TOK + 512
Y_ROWS = 384
Y_A, Y_B, Y_HF, Y_HB, Y_D, Y_O = 0, 64, 128, 192, 256, 320


def p_weight_cols():
    cols = []
    offs = {}
    for name, off, kind, _ in P_TILES:
        offs[name] = len(cols)
        cols.extend(range(off, off + 128))
        if kind.startswith("rope"):
            cols.extend((off + SW128).tolist())
    offs["cg"] = len(cols)
    cols.extend(range(O_CG, O_CG + 16))
    return np.array(cols), offs


P_COLS, P_OFFS = p_weight_cols()
NWP = len(P_COLS)
TBLOCKS = [(0, 512), (512, 512), (1024, 512), (1536, 512), (2048, 64)]
def phase_P(k, xsrc, xcol0, W, S1b, S1c):
    nc = k.nc
    with k.phase():
        ones = k.sb([128, 128], F32, "ones")
        bones = k.sb([128, 128], F32, "bones")
        k.op("pool", lambda e: e.memset(ones[:], 1.0), writes=[ones])
        k.op("pool", lambda e: e.memset(bones[:], 0.0), writes=[bones])
        k.op("pool", lambda e: e.memset(bones[0:64, 0:64], 1.0), writes=[bones])
        k.op("pool", lambda e: e.memset(bones[64:128, 64:128], 1.0), writes=[bones])
        bms = k.sb([128, 16], F32, "bms"); cvs = k.sb([128, 8, 2], F32, "cvs")
        g1s = k.sb([128, 8], F32, "g1s"); gns = k.sb([128, 4], F32, "gns")
        coss = k.sb([128, TC], F32, "coss"); sins = k.sb([128, TC], F32, "sins")
        k.dma("sp", bms[:], W["bm1"][:], writes=[bms]); k.dma("sp", cvs[:], W["cv"][:], writes=[cvs])
        k.dma("sp", g1s[:], W["g1"][:], writes=[g1s]); k.dma("sp", gns[:], W["gn"][:], writes=[gns])
        k.dma("sp", coss[:], W["cos"][:], writes=[coss]); k.dma("sp", sins[:], W["sin"][:], writes=[sins])
        k.op("dve", lambda e: e.tensor_scalar(gns[:, 0:2], gns[:, 0:2], 0.125, None, op0=ALU.mult), reads=[gns], writes=[gns])
        sc = k.sb([128, 8, 2], F32, "silu_c")
        k.op("act", lambda e: e.activation(out=sc[:], in_=cvs[:], func=AF.Silu), reads=[cvs], writes=[sc])
        mod = k.sb([128, 16, 2], F32, "mod")
        pmod = k.ps([128, 512], F32, "pmod")
        wmb = [k.sb([128, 8, 128], F32, "wmb") for i in range(2)]
        wm_v = W["wm1"].t.rearrange("(kc p) c -> p kc c", p=128)
        for j in range(16):
            wb_ = wmb[j % 2]
            k.dma("sp", wb_[:], wm_v[:, :, j * 128:(j + 1) * 128], writes=[wb_])
            for kc in range(8):
                k.op("pe", lambda e, kc=kc, wb_=wb_: e.matmul(pmod[:, 0:2], lhsT=wb_[:, kc, :], rhs=sc[:, kc, :], start=(kc == 0), stop=(kc == 7)),
                     reads=[wb_, sc], writes=[pmod])
            k.op("dve", lambda e, j=j: e.tensor_scalar(mod[:, j, :], pmod[:, 0:2], bms[:, j:j + 1], None, op0=ALU.add), reads=[pmod, bms], writes=[mod.p(j)])
        Asc = k.sb([128, 8, 2], F32, "Asc")
        k.op("dve", lambda e: e.tensor_scalar(Asc[:], mod[:, 8:16, :], 1.0, None, op0=ALU.add), reads=[mod], writes=[Asc])
        for w_ in range(2):
            k.op("dve", lambda e, w_=w_: e.tensor_tensor(out=Asc[:, :, w_], in0=Asc[:, :, w_], in1=g1s[:], op=ALU.mult), reads=[Asc, g1s], writes=[Asc])

        hT = k.sb([128, 8, TC], BF16, "hT")
        xb = [k.sb([128, 8, 512], F32, "xb") for i in range(2)]
        sq = k.sb([128, 8, 512], F32, "sq")
        pss = k.ps([128, 512], F32, "pss")
        rstd = k.sb([128, 512], F32, "rstd")
        xn = k.sb([128, 512], F32, "xn")
        xw = xsrc.t.shape[1]
        xT_v = xsrc.t.rearrange("(kc p) t -> p kc t", p=128)
        for bi, (t0, tn) in enumerate(TBLOCKS):
            xbb = xb[bi % 2]
            which = 1 if bi == 4 else 0
            s0 = (xw - TCX) if bi == 4 else (xcol0 + t0)
            k.dma("sp", xbb[:, :, 0:tn], xT_v[:, :, s0:s0 + tn], reads=[xsrc], writes=[xbb])
            k.op("act", lambda e, xbb=xbb, tn=tn: e.activation(out=sq[:, :, 0:tn], in_=xbb[:, :, 0:tn], func=AF.Square), reads=[xbb], writes=[sq])
            for kc in range(8):
                k.op("pe", lambda e, kc=kc, tn=tn: e.matmul(pss[:, 0:tn], lhsT=ones[:], rhs=sq[:, kc, 0:tn], start=(kc == 0), stop=(kc == 7)), reads=[ones, sq], writes=[pss])
            k.op("act", lambda e, tn=tn: e.activation(out=rstd[:, 0:tn], in_=pss[:, 0:tn], func=AF.Sqrt, scale=1.0 / D, bias=EPS), reads=[pss], writes=[rstd])
            k.op("dve", lambda e, tn=tn: e.reciprocal(rstd[:, 0:tn], rstd[:, 0:tn]), reads=[rstd], writes=[rstd])
            for kc in range(8):
                k.op("dve", lambda e, kc=kc, tn=tn, xbb=xbb: e.tensor_tensor(out=xn[:, 0:tn], in0=xbb[:, kc, 0:tn], in1=rstd[:, 0:tn], op=ALU.mult), reads=[xbb, rstd], writes=[xn])
                k.op("act", lambda e, kc=kc, tn=tn, t0=t0, which=which: e.activation(out=hT[:, kc, t0:t0 + tn], in_=xn[:, 0:tn], func=AF.Identity,
                                                                                 scale=Asc[:, kc, which:which + 1], bias=mod[:, kc, which:which + 1]),
                     reads=[xn, Asc, mod], writes=[hT.p(bi)])

        wst = [k.sb([128, 8, 256], F32, "wst") for i in range(2)]
        wbf = [k.sb([128, 8, 256], BF16, "wbf") for i in range(2)]
        px = [k.ps([128, 512], F32, "px") for i in range(2)]
        pxs = [k.ps([128, 512], F32, "pxs") for i in range(2)]
        pn = k.ps([128, 512], F32, "pn")
        ob = [k.sb([128, 512], BF16, "ob") for i in range(2)]
        of = [k.sb([128, 512], F32, "of") for i in range(2)]
        e1 = k.sb([128, 512], F32, "e1"); e2 = k.sb([128, 512], F32, "e2"); e3 = k.sb([128, 512], F32, "e3")
        rs2 = k.sb([128, 512], F32, "rs2")
        wp_v = W["wp"].t.rearrange("(kc p) c -> p kc c", p=128)
        it = 0
        for ti, (name, off, kind, (okind, orow)) in enumerate(P_TILES + [("cg", O_CG, "gates", ("g", 0))]):
            rope = kind.startswith("rope")
            ncol = 256 if rope else (16 if kind == "gates" else 128)
            c0 = P_OFFS[name]
            ws_, wb_ = wst[ti % 2], wbf[ti % 2]
            k.dma("sp", ws_[:, :, 0:ncol], wp_v[:, :, c0:c0 + ncol], writes=[ws_])
            k.op("pool", lambda e, ws_=ws_, wb_=wb_, ncol=ncol: e.tensor_copy(wb_[:, :, 0:ncol], ws_[:, :, 0:ncol]), reads=[ws_], writes=[wb_])
            M = 16 if kind == "gates" else 128
            if kind == "tok":
                dbuf = S1b if okind == "bv" else S1c
                base = (B_FM if okind == "bv" else C_TOK) * TC + orow * TC * 128
                for g0 in range(0, 17, 4):
                    subs = list(range(g0, min(g0 + 4, 17)))
                    pX = px[it % 2]
                    o_ = (ob if okind == "bv" else of)[it % 2]
                    for si, s in enumerate(subs):
                        tn = 128 if s < 16 else 64
                        for kc in range(8):
                            k.op("pe", lambda e, kc=kc, pX=pX, wb_=wb_, si=si, s=s, tn=tn: e.matmul(
                                pX[0:tn, si * 128:(si + 1) * 128], lhsT=hT[:, kc, s * 128:s * 128 + tn], rhs=wb_[:, kc, 0:128], start=(kc == 0), stop=(kc == 7)),
                                reads=[wb_, hT.p(min(s // 4, 4))], writes=[pX])
                    nfull = len([s for s in subs if s < 16])
                    if nfull:
                        k.op("act", lambda e, pX=pX, o_=o_, nfull=nfull: e.activation(out=o_[:, 0:nfull * 128], in_=pX[:, 0:nfull * 128], func=AF.Identity), reads=[pX], writes=[o_])
                        dst = bass.AP(dbuf.t, base + g0 * 128 * 128, [[128, 128], [128 * 128, nfull], [1, 128]])
                        k.dma("pool", dst, o_[:, 0:nfull * 128].rearrange("p (s c) -> p s c", c=128), reads=[o_], writes=[dbuf.p((ti, g0))])
                    if 16 in subs:
                        si = subs.index(16)
                        o2 = (ob if okind == "bv" else of)[(it + 1) % 2]
                        k.op("act", lambda e, pX=pX, o2=o2, si=si: e.activation(out=o2[0:64, 0:128], in_=pX[0:64, si * 128:(si + 1) * 128], func=AF.Identity), reads=[pX], writes=[o2])
                        dst = bass.AP(dbuf.t, base + 2048 * 128, [[128, 64], [1, 128]])
                        k.dma("pool", dst, o2[0:64, 0:128], reads=[o2], writes=[dbuf.p((ti, 99))])
                    it += 1
                continue
            for bi, (t0, tn) in enumerate(TBLOCKS):
                pX, pXs = px[it % 2], pxs[it % 2]
                for kc in range(8):
                    k.op("pe", lambda e, kc=kc, pX=pX, wb_=wb_, M=M, t0=t0, tn=tn: e.matmul(pX[0:M, 0:tn], lhsT=wb_[:, kc, 0:M], rhs=hT[:, kc, t0:t0 + tn], start=(kc == 0), stop=(kc == 7)),
                         reads=[wb_, hT.p(bi)], writes=[pX])
                if rope:
                    for kc in range(8):
                        k.op("pe", lambda e, kc=kc, pXs=pXs, wb_=wb_, t0=t0, tn=tn: e.matmul(pXs[:, 0:tn], lhsT=wb_[:, kc, 128:256], rhs=hT[:, kc, t0:t0 + tn], start=(kc == 0), stop=(kc == 7)),
                             reads=[wb_, hT.p(bi)], writes=[pXs])
                if okind == "b":
                    o_ = ob[it % 2]; dbuf = S1b; r0 = orow * 128
                elif okind == "c":
                    o_ = of[it % 2]; dbuf = S1c; r0 = orow * 128
                else:
                    o_ = of[it % 2]; dbuf = S1c; r0 = C_G
                dst = dbuf.t[r0:r0 + M, t0:t0 + tn]
                if rope:
                    isA = kind.startswith("ropeA")
                    isq = kind.endswith("_q")
                    if isA:
                        g0 = 0 if isq else 2
                        s1, s2 = gns[:, g0:g0 + 1], gns[:, g0 + 1:g0 + 2]
                    else:
                        s1 = s2 = (0.125 if isq else 1.0)
                    k.op("act", lambda e, pX=pX, tn=tn, s1=s1: e.activation(out=e1[:, 0:tn], in_=pX[:, 0:tn], func=AF.Identity, scale=s1), reads=[pX, gns], writes=[e1])
                    k.op("act", lambda e, pXs=pXs, tn=tn, s2=s2: e.activation(out=e2[:, 0:tn], in_=pXs[:, 0:tn], func=AF.Identity, scale=s2), reads=[pXs, gns], writes=[e2])
                    if isA:
                        k.op("act", lambda e, pX=pX, tn=tn: e.activation(out=e3[:, 0:tn], in_=pX[:, 0:tn], func=AF.Square), reads=[pX], writes=[e3])
                        k.op("pe", lambda e, tn=tn: e.matmul(pn[:, 0:tn], lhsT=bones[:], rhs=e3[:, 0:tn], start=True, stop=True), reads=[bones, e3], writes=[pn])
                        k.op("act", lambda e, tn=tn: e.activation(out=rs2[:, 0:tn], in_=pn[:, 0:tn], func=AF.Sqrt, scale=1.0 / 64, bias=EPS), reads=[pn], writes=[rs2])
                        k.op("dve", lambda e, tn=tn: e.reciprocal(rs2[:, 0:tn], rs2[:, 0:tn]), reads=[rs2], writes=[rs2])
                    k.op("dve", lambda e, tn=tn, t0=t0: e.tensor_tensor(out=e1[:, 0:tn], in0=e1[:, 0:tn], in1=coss[:, t0:t0 + tn], op=ALU.mult), reads=[e1, coss], writes=[e1])
                    k.op("dve", lambda e, tn=tn, t0=t0: e.tensor_tensor(out=e2[:, 0:tn], in0=e2[:, 0:tn], in1=sins[:, t0:t0 + tn], op=ALU.mult), reads=[e2, sins], writes=[e2])
                    if isA:
                        k.op("dve", lambda e, tn=tn: e.tensor_tensor(out=e1[:, 0:tn], in0=e1[:, 0:tn], in1=e2[:, 0:tn], op=ALU.add), reads=[e1, e2], writes=[e1])
                        k.op("dve", lambda e, tn=tn, o_=o_: e.tensor_tensor(out=o_[:, 0:tn], in0=e1[:, 0:tn], in1=rs2[:, 0:tn], op=ALU.mult), reads=[e1, rs2], writes=[o_])
                    else:
                        k.op("dve", lambda e, tn=tn, o_=o_: e.tensor_tensor(out=o_[:, 0:tn], in0=e1[:, 0:tn], in1=e2[:, 0:tn], op=ALU.add), reads=[e1, e2], writes=[o_])
                else:
                    scl = 0.125 if kind == "plain_q" else 1.0
                    k.op("act", lambda e, pX=pX, tn=tn, o_=o_, M=M, scl=scl: e.activation(out=o_[0:M, 0:tn], in_=pX[0:M, 0:tn], func=AF.Identity, scale=scl), reads=[pX], writes=[o_])
                k.dma("pool", dst, o_[0:M, 0:tn], reads=[o_], writes=[dbuf.p((ti, bi))])
                it += 1
def attn_schedules(need_ctx):
    sA, sB, sD = [], [], []
    if need_ctx:
        for s in (sA, sB, sD):
            s.append((0, 256, [(0, None), (1, None)]))
    for m in range(16):
        q0 = CTX + 512 * m
        sA.append((q0, 512, [(c, None) for c in range(NCHK)]))
        lb = []
        for r in range(6):
            n = 4 * m - 1 + r
            if 0 <= n < 64:
                lb.append((n + 2, r))
        sB.append((q0, 512, lb + [(0, None), (1, None)]))
        cls = 0 if m == 0 else (2 if m == 15 else 1)
        ld = []
        for o in range(8):
            n = 4 * m - 2 + o
            if 0 <= n < 64:
                ld.append((n + 2, 6 + cls * 8 + o))
        sD.append((q0, 512, ld + [(0, None), (1, None)]))
    return sA, sB, sD


def g1b_off(q, row, col):
    return (VB * 4 + q) * (B_ROWS * TC) + row * TC + col


def g1b_voff(q, vt, c0, tok0):
    return (VB * 4 + q) * (B_ROWS * TC) + B_FM * TC + (vt * TC + tok0) * 128 + c0


MA_SPEC = {
    "A": dict(qrow=VJ * 64, krow=VJH * 64 + 256, vt=Lin(0), vc=VJH * 64, yrow=Y_A),
    "B": dict(qrow=VJ * 64 + 384, krow=VJH * 64 + 640, vt=Lin(1), vc=VJH * 64, yrow=Y_B),
    "D": dict(qrow=VJ * 64 + 768, krow=VJ * 64 + 1024, vt=VJH + 2, vc=VJL * 64, yrow=Y_D),
}


def phase_MA(k, need_ctx, W, G1b, S2):
    with k.phase():
        QT = k.sb([64, TS], BF16, "QT"); KT = k.sb([64, TS], BF16, "KT")
        VA = k.sb([128, NCHK, 65], BF16, "VA")
        sinks = k.sb([128, 1], F32, "sinks"); esink = k.sb([128, 1], F32, "esink")
        sel = k.sb([65, 64], F32, "sel")
        k.op("pool", lambda e: e.memset(sel[:], 0.0), writes=[sel])
        k.op("pool", lambda e: e.memset(sel[64:65, :], 1.0), writes=[sel])
        k.dma("sp", sinks[:], W["sink"][:], writes=[sinks])
        k.op("act", lambda e: e.activation(out=esink[:], in_=sinks[:], func=AF.Exp), reads=[sinks], writes=[esink])
        pS = [k.ps([128, 512], F32, "pS") for i in range(3)]
        pO = [k.ps([128, 512], F32, "pO") for i in range(2)]
        pD = k.ps([128, 512], F32, "pD")
        PT = [k.sb([128, 512], BF16, "PT") for i in range(4)]
        bb = [k.sb([128, 512], F32, "bb") for i in range(3)]
        sbuf = [k.sb([128, 512], F32, "sbf") for i in range(2)]
        ot = [k.sb([65, 512], F32, "ot") for i in range(2)]
        rr = k.sb([64, 512], F32, "rr")
        yo = [k.sb([64, 512], F32, "yo") for i in range(2)]
        scheds = dict(zip("ABD", attn_schedules(need_ctx)))
        ib = [0]
        iq = 0
        for X in "ABD":
            sp_ = MA_SPEC[X]
            for q in range(4):
                k.dma_dyn(QT[:, CTX + TL * q:CTX + TL * (q + 1)], G1b, g1b_off(q, sp_["qrow"], 0), [[TC, 64], [1, TL]], writes=[QT.p(("l", q))])
                k.dma_dyn(QT[:, TCX * q:TCX * (q + 1)], G1b, g1b_off(q, sp_["qrow"], TL), [[TC, 64], [1, TCX]], writes=[QT.p(("c", q))])
                k.dma_dyn(KT[:, CTX + TL * q:CTX + TL * (q + 1)], G1b, g1b_off(q, sp_["krow"], 0), [[TC, 64], [1, TL]], writes=[KT.p(("l", q))])
                k.dma_dyn(KT[:, TCX * q:TCX * (q + 1)], G1b, g1b_off(q, sp_["krow"], TL), [[TC, 64], [1, TCX]], writes=[KT.p(("c", q))])
                for i0 in range(0, 16, 4):
                    k.dma_dyn(VA[:, 2 + 16 * q + i0:2 + 16 * q + i0 + 4, 0:64], G1b, g1b_voff(q, sp_["vt"], sp_["vc"], i0 * 128), [[128, 128], [128 * 128, 4], [1, 64]], writes=[VA.p(("l", q, i0))])
                k.dma_dyn(VA[(q % 2) * 64:(q % 2) * 64 + 64, q // 2, 0:64], G1b, g1b_voff(q, sp_["vt"], sp_["vc"], TL), [[128, 64], [1, 64]], writes=[VA.p(("c", q))])
            k.op("pool", lambda e: e.memset(VA[:, :, 64:65], 1.0), writes=[VA.p("ones")])
            tiles = []
            for qi, (q0, nq, chunks) in enumerate(scheds[X]):
                for ci, (c, bidx) in enumerate(chunks):
                    tiles.append((qi, q0, nq, ci, len(chunks), c, bidx))
            LA = 2

            def emit_S(ti):
                qi, q0, nq, ci, nchk, c, bidx = tiles[ti]
                ps = pS[ti % 3]
                pt = PT[ti % 4]
                k.op("pe", lambda e: e.matmul(ps[:, 0:nq], lhsT=KT[:, c * 128:(c + 1) * 128], rhs=QT[:, q0:q0 + nq], start=True, stop=True), reads=[KT, QT], writes=[ps])
                if bidx is not None:
                    bt = bb[ib[0] % 3]; ib[0] += 1
                    sbt = sbuf[ti % 2]
                    k.dma("sp", bt[:], W["bias"].t[bidx], writes=[bt])
                    k.op("dve", lambda e: e.tensor_tensor(out=sbt[:, 0:nq], in0=ps[:, 0:nq], in1=bt[:, 0:nq], op=ALU.add), reads=[ps, bt], writes=[sbt])
                    k.op("act", lambda e: e.activation(out=pt[:, 0:nq], in_=sbt[:, 0:nq], func=AF.Exp), reads=[sbt], writes=[pt])
                else:
                    k.op("act", lambda e: e.activation(out=pt[:, 0:nq], in_=ps[:, 0:nq], func=AF.Exp), reads=[ps], writes=[pt])
            for ti in range(min(LA, len(tiles))):
                emit_S(ti)
            for ti, (qi, q0, nq, ci, nchk, c, bidx) in enumerate(tiles):
                if ti + LA < len(tiles):
                    emit_S(ti + LA)
                po = pO[qi % 2]
                pt = PT[ti % 4]
                k.op("pe", lambda e, po=po, pt=pt, c=c, ci=ci, nchk=nchk, nq=nq: e.matmul(po[0:65, 0:nq], lhsT=VA[:, c, :], rhs=pt[:, 0:nq], start=(ci == 0), stop=(ci == nchk - 1)),
                     reads=[pt, VA], writes=[po])
                if ci != nchk - 1:
                    continue
                otb = ot[iq % 2]; yob = yo[iq % 2]; iq += 1
                k.op("act", lambda e, po=po, otb=otb, nq=nq: e.activation(out=otb[:, 0:nq], in_=po[0:65, 0:nq], func=AF.Identity), reads=[po], writes=[otb])
                k.op("pe", lambda e, otb=otb, nq=nq: e.matmul(pD[0:64, 0:nq], lhsT=sel[:, :], rhs=otb[:, 0:nq], start=True, stop=True), reads=[sel, otb], writes=[pD])
                if X == "B":
                    k.op("dve", lambda e, nq=nq: e.tensor_scalar(rr[:, 0:nq], pD[0:64, 0:nq], esink[0:64, 0:1], None, op0=ALU.add), reads=[pD, esink], writes=[rr])
                    k.op("dve", lambda e, nq=nq: e.reciprocal(rr[:, 0:nq], rr[:, 0:nq]), reads=[rr], writes=[rr])
                else:
                    k.op("dve", lambda e, nq=nq: e.reciprocal(rr[:, 0:nq], pD[0:64, 0:nq]), reads=[pD], writes=[rr])
                k.op("dve", lambda e, otb=otb, yob=yob, nq=nq: e.tensor_tensor(out=yob[:, 0:nq], in0=otb[0:64, 0:nq], in1=rr[:, 0:nq], op=ALU.mult), reads=[otb, rr], writes=[yob])
                r0 = sp_["yrow"]
                k.dma("sp", S2.t[r0:r0 + 64, 2 + q0:2 + q0 + nq], yob[:, 0:nq], reads=[yob], writes=[S2.p((X, q0))])
def g1c_off(q, row, col):
    return (VB * 4 + q) * (C_ROWS * TC) + row * TC + col


def g1c_toff(q, vt, tok0):
    return (VB * 4 + q) * (C_ROWS * TC) + C_TOK * TC + (vt * TC + tok0) * 128 + VJL * 64


MC_SEGS = [("c", 0, 4, 0), ("l", 0, 32, 4), ("l", 1, 32, 36), ("l", 2, 32, 68), ("l", 3, 32, 100)]
SEGC = 32


def phase_MC(k, W, G1c, S2):
    with k.phase():
        tri2 = k.sb([64, 2, 64], F32, "tri2"); ones = k.sb([64, 64], F32, "ones64")
        gb = k.sb([64, 4], F32, "gb"); nb = k.sb([64, 4], F32, "nb")
        sel = k.sb([65, 64], F32, "sel")
        k.op("pool", lambda e: e.memset(sel[:], 0.0), writes=[sel])
        k.op("pool", lambda e: e.memset(sel[64:65, :], 1.0), writes=[sel])
        k.dma("sp", tri2[:], W["tri"][:], writes=[tri2]); k.dma("sp", gb[:], W["gb"][:], writes=[gb])
        k.op("pool", lambda e: e.memset(ones[:], 1.0), writes=[ones])
        k.op("dve", lambda e: e.tensor_scalar(nb[:], gb[:], -1.0, None, op0=ALU.mult), reads=[gb], writes=[nb])
        for q in range(4):
            k.dma_dyn(S2.t[Y_O:Y_O + 64, 2 + CTX + TL * q:2 + CTX + TL * (q + 1)], G1c, g1c_off(q, VJ * 64 + 512, 0), [[TC, 64], [1, TL]], writes=[S2.p(("o", q))])
            k.dma_dyn(S2.t[Y_O:Y_O + 64, 2 + TCX * q:2 + TCX * (q + 1)], G1c, g1c_off(q, VJ * 64 + 512, TL), [[TC, 64], [1, TCX]], writes=[S2.p(("oc", q))])
        gates = k.sb([64, 4, NCH], F32, "gates")
        ident = k.sb([64, 64], F32, "ident")
        k.op("dve", lambda e: e.tensor_tensor(out=ident[:], in0=tri2[:, 0, :], in1=tri2[:, 1, :], op=ALU.mult), reads=[tri2], writes=[ident])
        graw = [k.sb([32, 64], F32, "graw") for i in range(2)]
        gctx = [k.sb([4, 64], F32, "gctx") for i in range(2)]
        pg = [k.ps([128, 512], F32, "pg") for i in range(1)]
        ig = 0
        for kind in range(4):
            for q in range(4):
                gr = graw[ig % 2]; ig += 1
                k.dma_dyn(gr[:], G1c, g1c_off(q, VJ + (C_G + kind * 4), 0), [[64, 32], [1, 64]], writes=[gr])
                k.op("pe", lambda e, gr=gr: e.matmul(pg[0][0:64, 0:32], lhsT=gr[:], rhs=ident[0:32, 0:32], start=True, stop=True), reads=[gr, ident], writes=[pg[0]])
                k.op("act", lambda e, kind=kind, q=q: e.activation(out=gates[:, kind, 4 + 32 * q:4 + 32 * (q + 1)], in_=pg[0][0:64, 0:32], func=AF.Identity), reads=[pg[0]], writes=[gates.p((kind, q))])
            gc = gctx[kind % 2]
            for q in range(4):
                k.dma_dyn(gc[q:q + 1, :], G1c, g1c_off(q, VJ + (C_G + kind * 4), TL), [[64, 1], [1, 64]], writes=[gc.p(q)])
            k.op("pe", lambda e, gc=gc: e.matmul(pg[0][0:64, 0:4], lhsT=gc[:], rhs=ident[0:4, 0:4], start=True, stop=True), reads=[gc, ident], writes=[pg[0]])
            k.op("act", lambda e, kind=kind: e.activation(out=gates[:, kind, 0:4], in_=pg[0][0:64, 0:4], func=AF.Identity), reads=[pg[0]], writes=[gates.p((kind, "c"))])
        li = k.sb([64, NCH], F32, "li"); sp = k.sb([64, NCH], F32, "sp"); bcol = k.sb([64, NCH], F32, "bcol")
        W_ = SEGC * 64
        qT = k.sb([64, W_], F32, "qT"); kT = k.sb([64, W_], F32, "kT"); ktok = k.sb([64, W_], F32, "ktok")
        vaug = k.sb([64, SEGC * 65], F32, "vaug")
        Z = k.sb([64, W_], F32, "Z"); Ab = k.sb([64, W_], F32, "Ab"); Wb = k.sb([64, W_], F32, "Wb")
        qw = k.sb([64, W_], F32, "qw"); kw = k.sb([64, W_], F32, "kw"); Dm = k.sb([64, W_], F32, "Dm"); SD = k.sb([64, W_], F32, "SD")
        U = k.sb([64, SEGC * 65], F32, "U"); CB = k.sb([64, SEGC * 65], F32, "CB"); carry = k.sb([64, 65], F32, "carry")
        wend = k.sb([64, SEGC], F32, "wend")
        ot = [k.sb([65, 512], F32, "ot") for i in range(2)]
        rr = k.sb([64, 512], F32, "rr"); hh = [k.sb([64, 512], F32, "hh") for i in range(2)]
        psb = [k.ps([128, 512], F32, "psb") for i in range(6)]
        pi = [0]

        def nextps():
            p = psb[pi[0] % 6]; pi[0] += 1
            return p
        vaug3 = vaug.t.rearrange("p (c e) -> p c e", e=65)
        ktok3 = ktok.t.rearrange("p (c e) -> p c e", e=64)
        Ab3 = Ab.t.rearrange("p (c t) -> p c t", t=64)
        k.op("pool", lambda e: e.memset(vaug3[:, :, 64:65], 1.0), writes=[vaug.p("ones")])
        alt = [0]

        def ve():
            alt[0] += 1
            return "dve" if alt[0] % 2 else "pool"
        io = 0
        for di in range(2):
            tri = tri2.t[:, di, :]
            fc = 63 if di == 0 else 0
            gi = gates.t[:, 2 * di, :]; gf = gates.t[:, 2 * di + 1, :]
            yrow = Y_HF if di == 0 else Y_HB
            k.op("dve", lambda e: e.tensor_scalar(li[:], gi, gb[:, 2 * di:2 * di + 1], None, op0=ALU.add), reads=[gates, gb], writes=[li])
            k.op("act", lambda e: e.activation(out=sp[:], in_=gf, func=AF.Exp, scale=-1.0, bias=nb[:, 2 * di + 1:2 * di + 2]), reads=[gates, nb], writes=[sp])
            k.op("act", lambda e: e.activation(out=sp[:], in_=sp[:], func=AF.Ln, scale=1.0, bias=1.0), reads=[sp], writes=[sp])
            pA = nextps()
            k.op("pe", lambda e: e.matmul(pA[0:64, 0:NCH], lhsT=tri, rhs=sp[:], start=True, stop=True), reads=[tri2, sp], writes=[pA])
            k.op("dve", lambda e: e.tensor_tensor(out=bcol[:], in0=pA[0:64, 0:NCH], in1=li[:], op=ALU.add), reads=[pA, li], writes=[bcol])
            k.op("dve", lambda e: e.memset(carry[:], 0.0), writes=[carry])
            segs = MC_SEGS if di == 0 else [MC_SEGS[0]] + MC_SEGS[:0:-1]
            for (skind, sq_, n, c0) in segs:
                Wn = n * 64
                if skind == "l":
                    q = sq_
                    k.dma_dyn(qT[:, 0:Wn], G1c, g1c_off(q, VJ * 64, 0), [[TC, 64], [1, TL]], writes=[qT])
                    k.dma_dyn(kT[:, 0:Wn], G1c, g1c_off(q, VJ * 64 + 256, 0), [[TC, 64], [1, TL]], writes=[kT])
                    for i0 in range(0, n, 8):
                        k.dma_dyn(ktok3[:, i0:i0 + 8, :], G1c, g1c_toff(q, VJH, i0 * 64), [[128, 64], [64 * 128, 8], [1, 64]], writes=[ktok.p(i0)])
                        k.dma_dyn(vaug3[:, i0:i0 + 8, 0:64], G1c, g1c_toff(q, VJH + 2, i0 * 64), [[128, 64], [64 * 128, 8], [1, 64]], writes=[vaug.p(("v", i0))])
                else:
                    for q in range(4):
                        k.dma_dyn(qT[:, q * 64:(q + 1) * 64], G1c, g1c_off(q, VJ * 64, TL), [[TC, 64], [1, 64]], writes=[qT.p(q)])
                        k.dma_dyn(kT[:, q * 64:(q + 1) * 64], G1c, g1c_off(q, VJ * 64 + 256, TL), [[TC, 64], [1, 64]], writes=[kT.p(q)])
                        k.dma_dyn(ktok3[:, q, :], G1c, g1c_toff(q, VJH, TL), [[128, 64], [1, 64]], writes=[ktok.p(q)])
                        k.dma_dyn(vaug3[:, q, 0:64], G1c, g1c_toff(q, VJH + 2, TL), [[128, 64], [1, 64]], writes=[vaug.p(("v", q))])
                for cc in range(n):
                    k.op(ve(), lambda e, cc=cc: e.tensor_scalar(Z[:, cc * 64:(cc + 1) * 64], tri, sp[:, c0 + cc:c0 + cc + 1], None, op0=ALU.mult), reads=[tri2, sp], writes=[Z.p(cc // 8)])
                for g0 in range(0, n, 8):
                    m = min(8, n - g0)
                    pz = nextps()
                    k.op("pe", lambda e, pz=pz, g0=g0, m=m: e.matmul(pz[0:64, 0:m * 64], lhsT=ones[:], rhs=Z[:, g0 * 64:(g0 + m) * 64], start=True, stop=True), reads=[ones, Z.p(g0 // 8)], writes=[pz])
                    k.op("act", lambda e, pz=pz, g0=g0, m=m: e.activation(out=Ab[:, g0 * 64:(g0 + m) * 64], in_=pz[0:64, 0:m * 64], func=AF.Identity, scale=-1.0), reads=[pz], writes=[Ab.p(g0 // 8)])
                k.op("act", lambda e: e.activation(out=Wb[:, 0:Wn], in_=Ab[:, 0:Wn], func=AF.Exp), reads=[Ab], writes=[Wb])
                k.op("dve", lambda e: e.tensor_tensor(out=qw[:, 0:Wn], in0=qT[:, 0:Wn], in1=Wb[:, 0:Wn], op=ALU.mult), reads=[qT, Wb], writes=[qw])
                k.op("dve", lambda e: e.tensor_tensor(out=wend[:, 0:n], in0=bcol[:, c0:c0 + n], in1=Ab3[:, 0:n, fc], op=ALU.add), reads=[bcol, Ab], writes=[wend])
                k.op("act", lambda e: e.activation(out=wend[:, 0:n], in_=wend[:, 0:n], func=AF.Exp), reads=[wend], writes=[wend])
                for cc in range(n):
                    k.op(ve(), lambda e, cc=cc: e.tensor_scalar(kw[:, cc * 64:(cc + 1) * 64], ktok[:, cc * 64:(cc + 1) * 64], wend[:, cc:cc + 1], None, op0=ALU.mult), reads=[ktok, wend], writes=[kw.p(cc // 7)])
                for g0 in range(0, n, 7):
                    m = min(7, n - g0)
                    pu = nextps()
                    for i in range(m):
                        cc = g0 + i
                        k.op("pe", lambda e, pu=pu, i=i, cc=cc: e.matmul(pu[0:64, i * 65:(i + 1) * 65], lhsT=kw[:, cc * 64:(cc + 1) * 64], rhs=vaug[:, cc * 65:(cc + 1) * 65], start=True, stop=True),
                             reads=[kw.p(cc // 7), vaug], writes=[pu])
                    k.op("act", lambda e, pu=pu, g0=g0, m=m: e.activation(out=U[:, g0 * 65:(g0 + m) * 65], in_=pu[0:64, 0:m * 65], func=AF.Identity), reads=[pu], writes=[U.p(g0 // 7)])
                order = list(range(n)) if di == 0 else list(range(n - 1, -1, -1))
                first = order[0]
                k.op("dve", lambda e, first=first: e.tensor_copy(CB[:, first * 65:(first + 1) * 65], carry[:]), reads=[carry], writes=[CB.p(first)])
                for oi, cc in enumerate(order):
                    nxt = order[oi + 1] if oi + 1 < n else None
                    dst = CB[:, nxt * 65:(nxt + 1) * 65] if nxt is not None else carry[:]
                    wr = CB.p(nxt) if nxt is not None else carry
                    k.op("dve", lambda e, cc=cc, dst=dst: e.scalar_tensor_tensor(out=dst, in0=CB[:, cc * 65:(cc + 1) * 65], scalar=Wb[:, cc * 64 + fc:cc * 64 + fc + 1],
                                                                                in1=U[:, cc * 65:(cc + 1) * 65], op0=ALU.mult, op1=ALU.add),
                         reads=[CB.p(cc), Wb, U.p(cc // 7)], writes=[wr])
                for cc in range(n):
                    k.op("act", lambda e, cc=cc: e.activation(out=Dm[:, cc * 64:(cc + 1) * 64], in_=Ab[:, cc * 64:(cc + 1) * 64], func=AF.Exp, bias=bcol[:, c0 + cc:c0 + cc + 1], scale=1.0),
                         reads=[Ab, bcol], writes=[Dm.p(cc // 8)])
                for g0 in range(0, n, 8):
                    m = min(8, n - g0)
                    pS = nextps()
                    for i in range(m):
                        cc = g0 + i
                        k.op("pe", lambda e, pS=pS, i=i, cc=cc: e.matmul(pS[0:64, i * 64:(i + 1) * 64], lhsT=kT[:, cc * 64:(cc + 1) * 64], rhs=qT[:, cc * 64:(cc + 1) * 64], start=True, stop=True),
                             reads=[kT, qT], writes=[pS])
                    k.op("dve", lambda e, pS=pS, g0=g0, m=m: e.tensor_tensor(out=SD[:, g0 * 64:(g0 + m) * 64], in0=pS[0:64, 0:m * 64], in1=Dm[:, g0 * 64:(g0 + m) * 64], op=ALU.mult),
                         reads=[pS, Dm.p(g0 // 8)], writes=[SD.p(g0 // 8)])
                    for i in range(m):
                        cc = g0 + i
                        k.op("pool", lambda e, cc=cc: e.tensor_tensor(out=SD[:, cc * 64:(cc + 1) * 64], in0=SD[:, cc * 64:(cc + 1) * 64], in1=tri, op=ALU.mult),
                             reads=[SD.p(g0 // 8), tri2], writes=[SD.p(g0 // 8)])
                    pN = nextps()
                    for i in range(m):
                        cc = g0 + i
                        k.op("pe", lambda e, pN=pN, i=i, cc=cc: e.matmul(pN[0:65, i * 64:(i + 1) * 64], lhsT=vaug[:, cc * 65:(cc + 1) * 65], rhs=SD[:, cc * 64:(cc + 1) * 64], start=True, stop=False),
                             reads=[SD.p(g0 // 8), vaug], writes=[pN])
                        k.op("pe", lambda e, pN=pN, i=i, cc=cc: e.matmul(pN[0:65, i * 64:(i + 1) * 64], lhsT=CB[:, cc * 65:(cc + 1) * 65], rhs=qw[:, cc * 64:(cc + 1) * 64], start=False, stop=True),
                             reads=[qw, CB.p(cc)], writes=[pN])
                    otb = ot[io % 2]; hb_ = hh[io % 2]; io += 1
                    mw = m * 64
                    k.op("act", lambda e, pN=pN, otb=otb, mw=mw: e.activation(out=otb[:, 0:mw], in_=pN[0:65, 0:mw], func=AF.Identity), reads=[pN], writes=[otb])
                    pD = nextps()
                    k.op("pe", lambda e, pD=pD, otb=otb, mw=mw: e.matmul(pD[0:64, 0:mw], lhsT=sel[:, :], rhs=otb[:, 0:mw], start=True, stop=True), reads=[sel, otb], writes=[pD])
                    k.op("act", lambda e, pD=pD, mw=mw: e.activation(out=rr[:, 0:mw], in_=pD[0:64, 0:mw], func=AF.Abs), reads=[pD], writes=[rr])
                    k.op("dve", lambda e, mw=mw: e.tensor_scalar(rr[:, 0:mw], rr[:, 0:mw], 1.0, None, op0=ALU.max), reads=[rr], writes=[rr])
                    k.op("dve", lambda e, mw=mw: e.reciprocal(rr[:, 0:mw], rr[:, 0:mw]), reads=[rr], writes=[rr])
                    k.op("dve", lambda e, otb=otb, hb_=hb_, mw=mw: e.tensor_tensor(out=hb_[:, 0:mw], in0=otb[0:64, 0:mw], in1=rr[:, 0:mw], op=ALU.mult), reads=[otb, rr], writes=[hb_])
                    s0 = 2 + (c0 + g0) * 64
                    k.dma("sp", S2.t[yrow:yrow + 64, s0:s0 + mw], hb_[:, 0:mw], reads=[hb_], writes=[S2.p((di, c0 + g0))])
def f_segments(layer):
    segs = []
    if layer == 0:
        o = 0
        for n in (513, 512, 512, 513):
            segs.append(dict(n=n, which=0, xcol=o, tok=o - 2, ooff=o))
            o += n
        segs.append(dict(n=TCX, which=1, xcol=2052, tok=-1, ooff=2050))
    else:
        for s in range(4):
            segs.append(dict(n=512, which=0, xcol=512 * s, tok=512 * s - 1, ooff=512 * s))
    hc = 0
    tc = 0
    for sg in segs:
        sg["hc"] = hc
        sg["tc"] = tc
        hc += sg["n"] + 2
        tc += sg["n"]
    return segs


def seg_flag_vals(layer, q):
    fl = []
    for sg in f_segments(layer):
        n = sg["n"]
        if sg["which"] == 0:
            base, smax = q * TL + sg["tok"], SEQ
        else:
            base, smax = q * TCX + sg["tok"], CTX
        for col in (0, 1, n, n + 1):
            fl.append(1.0 if 0 <= base + col < smax else 0.0)
    return fl


def g2_off(j, row, which, tok):
    base = (VB * 4 + j) * (Y_ROWS * YW) + row * YW
    if which == 0:
        return base + VJ * TL + (2 + CTX + tok)
    return base + VJ * TCX + (2 + tok)


YB_ROWS = [Y_A, Y_A, Y_B, Y_B, Y_HF, Y_HF, Y_HB, Y_HB, Y_O, Y_O, Y_D, Y_D]


def phase_F(k, layer, W, xsrc, G2, xdst, final, x1d):
    segs = f_segments(layer)
    NH = segs[-1]["hc"] + segs[-1]["n"] + 2
    NT = segs[-1]["tc"] + segs[-1]["n"]
    GT1, SH2, SC2, GT2 = 0, 8, 16, 24
    allfl = [seg_flag_vals(layer, q) for q in range(4)]
    need_flag = [any(allfl[q][i] == 0.0 for q in range(4)) for i in range(4 * len(segs))]
    BW = 258
    with k.phase():
        ones = k.sb([128, 128], F32, "ones")
        k.op("pool", lambda e: e.memset(ones[:], 1.0), writes=[ones])
        vs = k.sb([128, 24], F32, "vs"); cws = k.sb([128, 2 * NFC, 4], F32, "cws"); fls = k.sb([128, 20], F32, "fls")
        for a_, b_ in ((vs, W["vecs"]), (cws, W["cw"]), (fls, W["flg"])):
            k.dma("sp", a_[:], b_[:], writes=[a_])
        mod = k.sb([128, 32, 2], F32, "mod")
        A2 = k.sb([128, 8, 2], F32, "A2")
        h2all = k.sb([128, 8, NH], BF16, "h2all")
        with k.phase():
            bms = k.sb([128, 32], F32, "bms"); cvs = k.sb([128, 8, 2], F32, "cvs")
            k.dma("sp", bms[:], W["bm2"][:], writes=[bms]); k.dma("sp", cvs[:], W["cv"][:], writes=[cvs])
            sc = k.sb([128, 8, 2], F32, "silu_c")
            k.op("act", lambda e: e.activation(out=sc[:], in_=cvs[:], func=AF.Silu), reads=[cvs], writes=[sc])
            pmod = k.ps([128, 512], F32, "pmod")
            wmb = [k.sb([128, 8, 128], F32, "wmb") for i in range(2)]
            wm_v = W["wm2"].t.rearrange("(kc p) c -> p kc c", p=128)
            for j in range(32):
                wb_ = wmb[j % 2]
                k.dma("sp", wb_[:], wm_v[:, :, j * 128:(j + 1) * 128], writes=[wb_])
                for kc in range(8):
                    k.op("pe", lambda e, kc=kc, wb_=wb_: e.matmul(pmod[:, 0:2], lhsT=wb_[:, kc, :], rhs=sc[:, kc, :], start=(kc == 0), stop=(kc == 7)), reads=[wb_, sc], writes=[pmod])
                k.op("dve", lambda e, j=j: e.tensor_scalar(mod[:, j, :], pmod[:, 0:2], bms[:, j:j + 1], None, op0=ALU.add), reads=[pmod, bms], writes=[mod.p(j)])
            k.op("dve", lambda e: e.tensor_scalar(A2[:], mod[:, SC2:SC2 + 8, :], 1.0, None, op0=ALU.add), reads=[mod], writes=[A2])
            for w_ in range(2):
                k.op("dve", lambda e, w_=w_: e.tensor_tensor(out=A2[:, :, w_], in0=A2[:, :, w_], in1=vs[:, 8:16], op=ALU.mult), reads=[A2, vs], writes=[A2])
        with k.phase():
            wob = k.sb([128, 8, D], BF16, "wob")
            wos = [k.sb([128, 8, 128], F32, "wos") for i in range(2)]
            wo_v = W["wo"].t.rearrange("(kc p) c -> p kc c", p=128)
            for mc in range(8):
                s_ = wos[mc % 2]
                k.dma("sp", s_[:], wo_v[:, :, mc * 128:(mc + 1) * 128], writes=[s_])
                k.op("pool", lambda e, s_=s_, mc=mc: e.tensor_copy(wob[:, :, mc * 128:(mc + 1) * 128], s_[:]), reads=[s_], writes=[wob.p(mc)])
            yb = k.sb([128, 12, BW], F32, "yb"); y8 = k.sb([128, 8, BW], F32, "y8"); sq = k.sb([128, 8, BW], F32, "sq")
            yn = k.sb([128, 8, BW], BF16, "yn"); xb = k.sb([128, 8, BW], F32, "xb")
            x1 = [k.sb([128, 8, BW], F32, "x1") for i in range(2)]
            tmp = k.sb([128, BW], F32, "tmp"); rstd = k.sb([128, BW], F32, "rstd")
            pss = k.ps([128, 512], F32, "pss")
            pm = [k.ps([128, 512], F32, "pm") for i in range(3)]
            xT_v = xsrc.t.rearrange("(kc p) t -> p kc t", p=128)
            x1d_v = x1d.t.rearrange("(kc p) t -> p kc t", p=128)
            ibk = 0
            for si, sg_ in enumerate(segs):
                n, which, xcol, tok, hc = sg_["n"], sg_["which"], sg_["xcol"], sg_["tok"], sg_["hc"]
                ncol = n + 2
                blocks = [(0, ncol)] if ncol <= BW else [(0, ncol // 2), (ncol // 2, ncol - ncol // 2)]
                for (c0, cn) in blocks:
                    x1b = x1[ibk % 2]; ibk += 1
                    for ch in range(12):
                        for hf_ in range(2):
                            jh = 2 * (ch % 2) + hf_
                            k.dma_dyn(yb[hf_ * 64:(hf_ + 1) * 64, ch, 0:cn], G2, g2_off(jh, YB_ROWS[ch], which, tok + c0), [[YW, 64], [1, cn]], writes=[yb.p((ch, hf_))])
                    k.dma("sp", xb[:, :, 0:cn], xT_v[:, :, xcol + c0:xcol + c0 + cn], reads=[xsrc], writes=[xb])
                    k.op("pool", lambda e, cn=cn: e.tensor_copy(y8[:, 0:4, 0:cn], yb[:, 0:4, 0:cn]), reads=[yb], writes=[y8.p("ab")])
                    k.op("pool", lambda e, cn=cn: e.tensor_copy(y8[:, 6:8, 0:cn], yb[:, 10:12, 0:cn]), reads=[yb], writes=[y8.p("d")])
                    k.op("dve", lambda e, cn=cn: e.tensor_tensor(out=y8[:, 4:6, 0:cn], in0=yb[:, 4:6, 0:cn], in1=yb[:, 6:8, 0:cn], op=ALU.add), reads=[yb], writes=[y8.p("c")])
                    k.op("act", lambda e, cn=cn: e.activation(out=sq[:, 0:2, 0:cn], in_=yb[:, 8:10, 0:cn], func=AF.Sigmoid), reads=[yb], writes=[sq])
                    k.op("dve", lambda e, cn=cn: e.tensor_tensor(out=y8[:, 4:6, 0:cn], in0=y8[:, 4:6, 0:cn], in1=sq[:, 0:2, 0:cn], op=ALU.mult), reads=[y8.p("c"), sq], writes=[y8.p("c")])
                    k.op("act", lambda e, cn=cn: e.activation(out=sq[:, :, 0:cn], in_=y8[:, :, 0:cn], func=AF.Square), reads=[y8], writes=[sq])
                    for g in range(4):
                        for i in range(2):
                            k.op("pe", lambda e, g=g, i=i, cn=cn: e.matmul(pss[:, 0:cn], lhsT=ones[:], rhs=sq[:, 2 * g + i, 0:cn], start=(i == 0), stop=(i == 1)), reads=[ones, sq], writes=[pss])
                        k.op("act", lambda e, cn=cn: e.activation(out=rstd[:, 0:cn], in_=pss[:, 0:cn], func=AF.Sqrt, scale=1.0 / 256, bias=EPS), reads=[pss], writes=[rstd])
                        k.op("dve", lambda e, cn=cn: e.reciprocal(rstd[:, 0:cn], rstd[:, 0:cn]), reads=[rstd], writes=[rstd])
                        for i in range(2):
                            kc = 2 * g + i
                            k.op("dve", lambda e, kc=kc, cn=cn: e.tensor_tensor(out=tmp[:, 0:cn], in0=y8[:, kc, 0:cn], in1=rstd[:, 0:cn], op=ALU.mult), reads=[y8, rstd], writes=[tmp])
                            k.op("act", lambda e, kc=kc, cn=cn: e.activation(out=yn[:, kc, 0:cn], in_=tmp[:, 0:cn], func=AF.Identity, scale=vs[:, kc:kc + 1]), reads=[tmp, vs], writes=[yn.p(kc)])
                    for mc in range(8):
                        p_ = pm[mc % 3]
                        for kc in range(8):
                            k.op("pe", lambda e, p_=p_, kc=kc, mc=mc, cn=cn: e.matmul(p_[:, 0:cn], lhsT=wob[:, kc, mc * 128:(mc + 1) * 128], rhs=yn[:, kc, 0:cn], start=(kc == 0), stop=(kc == 7)),
                                 reads=[wob.p(mc), yn], writes=[p_])
                        k.op("dve", lambda e, p_=p_, mc=mc, cn=cn, x1b=x1b: e.scalar_tensor_tensor(out=x1b[:, mc, 0:cn], in0=p_[:, 0:cn], scalar=mod[:, GT1 + mc, which:which + 1],
                                                                                            in1=xb[:, mc, 0:cn], op0=ALU.mult, op1=ALU.add), reads=[p_, mod, xb], writes=[x1b.p(mc)])
                    k.dma("sp", x1d_v[:, :, hc + c0:hc + c0 + cn], x1b[:, :, 0:cn], reads=[x1b], writes=[x1d.p((si, c0))])
                    k.op("act", lambda e, cn=cn, x1b=x1b: e.activation(out=sq[:, :, 0:cn], in_=x1b[:, :, 0:cn], func=AF.Square), reads=[x1b], writes=[sq])
                    for kc in range(8):
                        k.op("pe", lambda e, kc=kc, cn=cn: e.matmul(pss[:, 0:cn], lhsT=ones[:], rhs=sq[:, kc, 0:cn], start=(kc == 0), stop=(kc == 7)), reads=[ones, sq], writes=[pss])
                    k.op("act", lambda e, cn=cn: e.activation(out=rstd[:, 0:cn], in_=pss[:, 0:cn], func=AF.Sqrt, scale=1.0 / D, bias=EPS), reads=[pss], writes=[rstd])
                    k.op("dve", lambda e, cn=cn: e.reciprocal(rstd[:, 0:cn], rstd[:, 0:cn]), reads=[rstd], writes=[rstd])
                    for kc in range(8):
                        k.op("dve", lambda e, kc=kc, cn=cn, x1b=x1b: e.tensor_tensor(out=tmp[:, 0:cn], in0=x1b[:, kc, 0:cn], in1=rstd[:, 0:cn], op=ALU.mult), reads=[x1b, rstd], writes=[tmp])
                        k.op("act", lambda e, kc=kc, cn=cn, c0=c0: e.activation(out=h2all[:, kc, hc + c0:hc + c0 + cn], in_=tmp[:, 0:cn], func=AF.Identity, scale=A2[:, kc, which:which + 1], bias=mod[:, SH2 + kc, which:which + 1]),
                             reads=[tmp, A2, mod], writes=[h2all.p((si, c0))])
        with k.phase():
            aT = k.sb([128, NFC, NT], BF16, "aTall")
            with k.phase():
                pu = [k.ps([128, 512], F32, "pu") for i in range(6)]
                wus = [k.sb([128, 8, 256], F32, "wus") for i in range(2)]
                wub = [k.sb([128, 8, 256], BF16, "wub") for i in range(2)]
                ug = [k.sb([128, 516], F32, "ug") for i in range(2)]; uv = [k.sb([128, 516], F32, "uv") for i in range(2)]
                tg = [k.sb([128, 513], F32, "tg") for i in range(2)]; tv = [k.sb([128, 513], F32, "tv") for i in range(2)]
                sgl = [k.sb([128, 513], F32, "sgl") for i in range(2)]
                wu_v = W["wu"].t.rearrange("(kc p) c -> p kc c", p=128)
                it = 0
                ip = 0
                for fc in range(NFC):
                    ws_, wb_ = wus[fc % 2], wub[fc % 2]
                    k.dma("sp", ws_[:, :, 0:128], wu_v[:, :, fc * 128:(fc + 1) * 128], writes=[ws_.p(0)])
                    k.dma("sp", ws_[:, :, 128:256], wu_v[:, :, D_FF + fc * 128:D_FF + (fc + 1) * 128], writes=[ws_.p(1)])
                    k.op("pool", lambda e, ws_=ws_, wb_=wb_: e.tensor_copy(wb_[:], ws_[:]), reads=[ws_], writes=[wb_])
                    for si, sg_ in enumerate(segs):
                        n, hc, tc = sg_["n"], sg_["hc"], sg_["tc"]
                        ncol = n + 2
                        blocks = [(0, ncol)] if ncol <= BW else [(0, ncol // 2), (ncol // 2, ncol - ncol // 2)]
                        ug_, uv_, tg_, tv_, sgl_ = ug[it % 2], uv[it % 2], tg[it % 2], tv[it % 2], sgl[it % 2]
                        it += 1
                        for bi, (c0, cn) in enumerate(blocks):
                            pg_, pv_ = pu[ip % 6], pu[(ip + 1) % 6]
                            ip += 2
                            for kc in range(8):
                                k.op("pe", lambda e, pg_=pg_, kc=kc, wb_=wb_, c0=c0, cn=cn: e.matmul(pg_[:, 0:cn], lhsT=wb_[:, kc, 0:128], rhs=h2all[:, kc, hc + c0:hc + c0 + cn], start=(kc == 0), stop=(kc == 7)), reads=[wb_, h2all], writes=[pg_])
                            for kc in range(8):
                                k.op("pe", lambda e, pv_=pv_, kc=kc, wb_=wb_, c0=c0, cn=cn: e.matmul(pv_[:, 0:cn], lhsT=wb_[:, kc, 128:256], rhs=h2all[:, kc, hc + c0:hc + c0 + cn], start=(kc == 0), stop=(kc == 7)), reads=[wb_, h2all], writes=[pv_])
                            k.op("act", lambda e, pg_=pg_, c0=c0, cn=cn: e.activation(out=ug_[:, c0:c0 + cn], in_=pg_[:, 0:cn], func=AF.Identity), reads=[pg_], writes=[ug_])
                            k.op("act", lambda e, pv_=pv_, c0=c0, cn=cn: e.activation(out=uv_[:, c0:c0 + cn], in_=pv_[:, 0:cn], func=AF.Identity), reads=[pv_], writes=[uv_])
                        for u_ in (ug_, uv_):
                            for fi, col in enumerate((0, 1, ncol - 2, ncol - 1)):
                                if need_flag[4 * si + fi]:
                                    k.op("pool", lambda e, u_=u_, fi=fi, col=col: e.tensor_scalar(u_[:, col:col + 1], u_[:, col:col + 1], fls[:, 4 * si + fi:4 * si + fi + 1], None, op0=ALU.mult), reads=[u_, fls], writes=[u_])
                        for u_, t_, j in ((ug_, tg_, fc), (uv_, tv_, NFC + fc)):
                            k.op("dve", lambda e, u_=u_, t_=t_, j=j: e.tensor_scalar(t_[:, 0:n], u_[:, 0:n], cws[:, j, 0:1], None, op0=ALU.mult), reads=[u_, cws], writes=[t_])
                            k.op("dve", lambda e, u_=u_, t_=t_, j=j: e.scalar_tensor_tensor(out=t_[:, 0:n], in0=u_[:, 1:n + 1], scalar=cws[:, j, 1:2], in1=t_[:, 0:n], op0=ALU.mult, op1=ALU.add), reads=[u_, cws, t_], writes=[t_])
                            k.op("dve", lambda e, u_=u_, t_=t_, j=j: e.scalar_tensor_tensor(out=t_[:, 0:n], in0=u_[:, 2:n + 2], scalar=cws[:, j, 2:3], in1=t_[:, 0:n], op0=ALU.mult, op1=ALU.add), reads=[u_, cws, t_], writes=[t_])
                        k.op("act", lambda e, fc=fc: e.activation(out=sgl_[:, 0:n], in_=tg_[:, 0:n], func=AF.Silu, bias=cws[:, fc, 3:4], scale=1.0), reads=[tg_, cws], writes=[sgl_])
                        k.op("dve", lambda e, fc=fc: e.scalar_tensor_tensor(out=aT[:, fc, tc:tc + n], in0=tv_[:, 0:n], scalar=cws[:, NFC + fc, 3:4], in1=sgl_[:, 0:n], op0=ALU.add, op1=ALU.mult), reads=[tv_, cws, sgl_], writes=[aT.p((fc, si))])
            with k.phase():
                wdb = k.sb([128, NFC, D], BF16, "wdb")
                wds = [k.sb([128, 11, 128], F32, "wds") for i in range(2)]
                wd_v = W["wd"].t.rearrange("(fc p) c -> p fc c", p=128)
                iws = 0
                for mc in range(8):
                    for f0 in (0, 11):
                        ws_ = wds[iws % 2]; iws += 1
                        k.dma("sp", ws_[:], wd_v[:, f0:f0 + 11, mc * 128:(mc + 1) * 128], writes=[ws_])
                        k.op("pool", lambda e, ws_=ws_, mc=mc, f0=f0: e.tensor_copy(wdb[:, f0:f0 + 11, mc * 128:(mc + 1) * 128], ws_[:]), reads=[ws_], writes=[wdb.p((mc, f0))])
                xblk = [k.sb([128, 8, 512], F32, "xblk") for i in range(1)]
                sqm = k.sb([128, 512], F32, "sqm"); rs = k.sb([128, 512], F32, "rs"); tq = k.sb([128, 512], F32, "tq")
                ob = [k.sb([128, 512], F32, "ob") for i in range(2)]
                pm = [k.ps([128, 512], F32, "pm") for i in range(3)]
                pss = k.ps([128, 512], F32, "pss")
                x1d_v = x1d.t.rearrange("(kc p) t -> p kc t", p=128)
                xo_v = xdst.t.rearrange("(kc p) t -> p kc t", p=128)
                ib_ = 0
                io = 0
                for si, sg_ in enumerate(segs):
                    n, which, hc, tc, ooff = sg_["n"], sg_["which"], sg_["hc"], sg_["tc"], sg_["ooff"]
                    nb = (n + 511) // 512
                    bs = (n + nb - 1) // nb
                    for b0 in range(0, n, bs):
                        bn = min(bs, n - b0)
                        xb_ = xblk[0]; ib_ += 1
                        k.dma("sp", xb_[:, :, 0:bn], x1d_v[:, :, hc + 1 + b0:hc + 1 + b0 + bn], reads=[x1d], writes=[xb_])
                        for mc in range(8):
                            p_ = pm[mc % 3]
                            for fc in range(NFC):
                                k.op("pe", lambda e, p_=p_, fc=fc, mc=mc, b0=b0, bn=bn: e.matmul(p_[:, 0:bn], lhsT=wdb[:, fc, mc * 128:(mc + 1) * 128], rhs=aT[:, fc, tc + b0:tc + b0 + bn], start=(fc == 0), stop=(fc == NFC - 1)), reads=[wdb, aT], writes=[p_])
                            k.op("dve", lambda e, p_=p_, mc=mc, bn=bn, xb_=xb_: e.scalar_tensor_tensor(out=xb_[:, mc, 0:bn], in0=p_[:, 0:bn], scalar=mod[:, GT2 + mc, which:which + 1], in1=xb_[:, mc, 0:bn], op0=ALU.mult, op1=ALU.add),
                                 reads=[p_, mod, xb_.p(mc)], writes=[xb_.p(mc)])
                            if final:
                                k.op("act", lambda e, mc=mc, bn=bn, xb_=xb_: e.activation(out=sqm[:, 0:bn], in_=xb_[:, mc, 0:bn], func=AF.Square), reads=[xb_.p(mc)], writes=[sqm])
                                k.op("pe", lambda e, mc=mc, bn=bn: e.matmul(pss[:, 0:bn], lhsT=ones[:], rhs=sqm[:, 0:bn], start=(mc == 0), stop=(mc == 7)), reads=[ones, sqm], writes=[pss])
                        if not final:
                            k.dma("sp", xo_v[:, :, ooff + b0:ooff + b0 + bn], xb_[:, :, 0:bn], reads=[xb_], writes=[xdst.p((si, b0))])
                        else:
                            k.op("act", lambda e, bn=bn: e.activation(out=rs[:, 0:bn], in_=pss[:, 0:bn], func=AF.Sqrt, scale=1.0 / D, bias=EPS), reads=[pss], writes=[rs])
                            k.op("dve", lambda e, bn=bn: e.reciprocal(rs[:, 0:bn], rs[:, 0:bn]), reads=[rs], writes=[rs])
                            for mc in range(8):
                                o_ = ob[io % 2]; io += 1
                                k.op("dve", lambda e, mc=mc, bn=bn, xb_=xb_: e.tensor_tensor(out=tq[:, 0:bn], in0=xb_[:, mc, 0:bn], in1=rs[:, 0:bn], op=ALU.mult), reads=[xb_, rs], writes=[tq])
                                k.op("act", lambda e, mc=mc, o_=o_, bn=bn: e.activation(out=o_[:, 0:bn], in_=tq[:, 0:bn], func=AF.Identity, scale=vs[:, 16 + mc:17 + mc]), reads=[tq, vs], writes=[o_])
                                k.dma("sp", xdst.t[mc * 128:(mc + 1) * 128, ooff + b0:ooff + b0 + bn], o_[:, 0:bn], reads=[o_], writes=[xdst.p((si, b0, mc))], is_output=True)
LAYER_INPUTS = [("wm1", [D, 2048], F32), ("bm1", [128, 16], F32), ("g1", [128, 8], F32), ("wp", [D, NWP], F32), ("gn", [128, 4], F32),
                ("bias", [30, 128, 512], F32), ("sink", [128, 1], F32), ("gb", [64, 4], F32),
                ("wm2", [D, 4096], F32), ("bm2", [128, 32], F32), ("vecs", [128, 24], F32), ("wo", [D, D], F32), ("wu", [D, 2 * D_FF], F32),
                ("cw", [128, 2 * NFC, 4], F32), ("wd", [D_FF, D], F32), ("flg", [128, 20], F32)]
SHARED_INPUTS = [("cv", [128, 8, 2], F32), ("cos", [128, TC], F32), ("sin", [128, TC], F32), ("tri", [64, 2, 64], F32)]
XHW = 2052 + 66
XMW = 2050 + TCX


def build_fused():
    nc = bass.Bass("TRN2", target_bir_lowering=False)
    st = ExitStack()
    with st:
        k = KB(nc, st)
        xT = k.dram("xT", [D, TC], F32, "ExternalInput")
        xTh = k.dram("xTh", [D, XHW], F32, "ExternalInput")
        meta = k.dram("meta", [1, NMETA], I32, "ExternalInput")
        shared = {n: k.dram(n, s, d, "ExternalInput") for n, s, d in SHARED_INPUTS}
        Wl = []
        for l in range(2):
            w = {n: k.dram("%s_%d" % (n, l), s, d, "ExternalInput") for n, s, d in LAYER_INPUTS}
            w.update(shared)
            Wl.append(w)
        out = k.dram("out", [D, TL], F32, "ExternalOutput")
        S1b = [k.dram("S1b%d" % l, [B_ROWS, TC], BF16) for l in range(2)]
        S1c = [k.dram("S1c%d" % l, [C_ROWS, TC], F32) for l in range(2)]
        G1b = [k.dram("G1b%d" % l, [8 * B_ROWS, TC], BF16) for l in range(2)]
        G1c = [k.dram("G1c%d" % l, [8 * C_ROWS, TC], F32) for l in range(2)]
        S2 = k.dram("S2", [Y_ROWS, YW], F32)
        G2 = k.dram("G2", [8 * Y_ROWS, YW], F32)
        xmid = k.dram("xmid", [D, XMW], F32)
        x1d = k.dram("x1d", [D, 2200], F32)
        meta_sb = k.sb([1, NMETA], I32, "meta_sb")
        k.dma("sp", meta_sb[:], meta[:], writes=[meta_sb])
        k.load_coords(meta_sb)
        zt = k.sb([128, 2], F32, "zt")
        k.op("pool", lambda e: e.memset(zt[:], 0.0), writes=[zt])
        for r0 in range(0, Y_ROWS, 128):
            k.dma("sp", S2.t[r0:r0 + 128, 0:2], zt[:], reads=[zt], writes=[S2.p(("pad0", r0))])
            k.dma("sp", S2.t[r0:r0 + 128, TS + 2:TS + 4], zt[:], reads=[zt], writes=[S2.p(("pad1", r0))])
        for l in range(2):
            xsrc, xcol0 = (xT, 0) if l == 0 else (xmid, 1)
            phase_P(k, xsrc, xcol0, Wl[l], S1b[l], S1c[l])
            k.all_gather(S1b[l], G1b[l])
            k.all_gather(S1c[l], G1c[l])
            phase_MA(k, l == 0, Wl[l], G1b[l], S2)
            phase_MC(k, Wl[l], G1c[l], S2)
            k.all_gather(S2, G2)
            if l == 0:
                phase_F(k, 0, Wl[l], xTh, G2, xmid, False, x1d)
            else:
                phase_F(k, 1, Wl[l], xmid, G2, out, True, x1d)
        k.finish()
    return nc


def rope_tables(q):
    t = np.arange(q * TL, (q + 1) * TL)
    row = (t // 64).astype(np.float32)
    col = (t % 64).astype(np.float32)
    freqs = (np.float32(10000.0) ** (-np.arange(16, dtype=np.float32) / 16)).astype(np.float32)
    d = np.arange(64)
    half = d // 32
    i = d % 32
    f = i % 16
    first = i < 16
    pos = np.where(half[:, None] == 0, row[None, :], col[None, :]).astype(np.float32)
    ang = pos * freqs[f][:, None]
    cos = np.cos(ang).astype(np.float32)
    sin = np.sin(ang).astype(np.float32)
    sin = np.where(first[:, None], -sin, sin)
    cos = np.concatenate([cos, np.ones((64, TCX), np.float32)], axis=1)
    sin = np.concatenate([sin, np.zeros((64, TCX), np.float32)], axis=1)
    return np.ascontiguousarray(np.tile(cos, (2, 1))), np.ascontiguousarray(np.tile(sin, (2, 1)))


def chunk_vec(v):
    return np.ascontiguousarray(v.reshape(-1, 128).T)


def b_mask_tiles():
    i = np.arange(128)[:, None]
    jq = np.arange(512)[None, :]
    return np.stack([np.where(np.abs(128 * (r - 1) + i - jq) <= 128, 0.0, NEG).astype(np.float32) for r in range(6)])


def d_bias_tiles(rpb_h):
    i = np.arange(128)[:, None]
    jq = np.arange(512)[None, :]
    out = np.full((3, 8, 128, 512), NEG, np.float32)
    for cls, m in enumerate((0, 5, 15)):
        for o in range(8):
            n = 4 * m - 2 + o
            if not (0 <= n < 64):
                continue
            kr = 2 * n + i // 64
            kc = i % 64
            qr = 8 * m + jq // 64
            qc = jq % 64
            rs = np.clip(qr - 4, 0, 120)
            cs = np.clip(qc - 8, 0, 48)
            valid = (kr >= rs) & (kr < rs + 8) & (kc >= cs) & (kc < cs + 16)
            dr = np.clip(kr - qr + 7, 0, 14)
            dc = np.clip(kc - qc + 15, 0, 30)
            out[cls, o] = np.where(valid, rpb_h[dr, dc], np.float32(NEG))
    return out.reshape(24, 128, 512)


def take_cols(arr, lo, n, smax):
    out = np.zeros((arr.shape[1], n), np.float32)
    a, b = max(lo, 0), min(lo + n, smax)
    out[:, a - lo:b - lo] = arr[a:b].T
    return out


def seg_flags(layer, q):
    fl = seg_flag_vals(layer, q)
    fl += [1.0] * (20 - len(fl))
    return np.tile(np.array(fl, np.float32)[None, :], (128, 1))


_CACHE = {}


def kernel(**inputs):
    inp = {k_: np.ascontiguousarray(np.asarray(v)) for k_, v in inputs.items()}
    if "prog" not in _CACHE:
        _CACHE["prog"] = build_fused()
    nc = _CACHE["prog"]
    bm = b_mask_tiles()
    tri = np.ascontiguousarray(np.stack([np.triu(np.ones((64, 64), np.float32)), np.tril(np.ones((64, 64), np.float32))], axis=1))
    lay = []
    for l in range(2):
        w_mod = inp["w_mod"][l]
        gq, gk = inp["a_q_gain"][l], inp["a_k_gain"][l]
        gn = np.tile(np.stack([gq, gq[SW64], gk, gk[SW64]], axis=1), (2, 1)).astype(np.float32)
        cwf = np.concatenate([inp["conv_w"][l], inp["conv_b"][l][None, :]], axis=0)
        lay.append({
            "wm1": np.ascontiguousarray(w_mod[:, 0:2048]), "bm1": chunk_vec(inp["b_mod"][l][0:2048]), "g1": chunk_vec(inp["g_norm1"][l]),
            "wp": np.ascontiguousarray(inp["w_in"][l][:, P_COLS]), "gn": np.ascontiguousarray(gn),
            "wm2": np.ascontiguousarray(w_mod[:, 2048:6144]), "bm2": chunk_vec(inp["b_mod"][l][2048:6144]),
            "vecs": np.ascontiguousarray(np.concatenate([chunk_vec(inp["g_group"][l]), chunk_vec(inp["g_norm2"][l]), chunk_vec(inp["g_final"])], axis=1)),
            "wo": inp["w_out"][l], "wu": inp["w_up"][l], "cw": np.ascontiguousarray(cwf.T.reshape(2 * NFC, 128, 4).transpose(1, 0, 2)), "wd": inp["w_down"][l],
        })
    maps = []
    for core in range(8):
        b, q = core // 4, core % 4
        j = q
        m = {}
        lo = q * TL
        m["xT"] = np.ascontiguousarray(np.concatenate([take_cols(inp["x"][b], lo, TL, SEQ), take_cols(inp["ctx"][b], q * TCX, TCX, CTX)], axis=1))
        m["xTh"] = np.ascontiguousarray(np.concatenate([take_cols(inp["x"][b], lo - 2, TL + 4, SEQ), take_cols(inp["ctx"][b], q * TCX - 1, TCX + 2, CTX)], axis=1))
        m["meta"] = np.array([[4 * b + j, 2 * b + j // 2, 0, 0, 0, 0, 0, 0]], np.int32)
        m["cv"] = np.ascontiguousarray(np.stack([chunk_vec(inp["c"][b]), chunk_vec(inp["c_ctx"])], axis=2))
        m["cos"], m["sin"] = rope_tables(q)
        m["tri"] = tri
        for l in range(2):
            for n_, v in lay[l].items():
                m["%s_%d" % (n_, l)] = v
            m["bias_%d" % l] = np.ascontiguousarray(np.concatenate([bm, d_bias_tiles(inp["d_rel_bias"][l][j])], axis=0))
            m["sink_%d" % l] = np.full((128, 1), inp["b_sink"][l][j], np.float32)
            gbv = inp["c_gate_bias"][l]
            m["gb_%d" % l] = np.ascontiguousarray(np.tile(np.array([gbv[j], gbv[4 + j], gbv[8 + j], gbv[12 + j]], np.float32)[None, :], (64, 1)))
            m["flg_%d" % l] = seg_flags(l, q)
        maps.append(m)
    res = run_bass_kernel_spmd(nc, maps, core_ids=list(range(8)))
    out = np.zeros((NB, SEQ, D), np.float32)
    for core in range(8):
        b, q = core // 4, core % 4
        out[b, q * TL:(q + 1) * TL, :] = np.asarray(res.results[core]["out"]).T
    return out
```

```python
import numpy as np
import concourse.bass as bass
import concourse.mybir as mybir
from concourse.bass_utils import run_bass_kernel_spmd
from contextlib import ExitStack

F32 = mybir.dt.float32
BF16 = mybir.dt.bfloat16
I32 = mybir.dt.int32
AF = mybir.ActivationFunctionType
ALU = mybir.AluOpType
NMETA = 8


class Lin:
    def __init__(self, c=0, **co):
        self.c = c
        self.co = {n: v for n, v in co.items() if v}

    def __add__(self, o):
        if isinstance(o, int):
            return Lin(self.c + o, **self.co)
        co = dict(self.co)
        for n, v in o.co.items():
            co[n] = co.get(n, 0) + v
        return Lin(self.c + o.c, **co)

    __radd__ = __add__

    def __mul__(self, m):
        return Lin(self.c * m, **{n: v * m for n, v in self.co.items()})

    __rmul__ = __mul__

    def expr(self, V):
        e = None
        for n, v in self.co.items():
            t = V[n] * v
            e = t if e is None else e + t
        return self.c if e is None else e + self.c

    def at(self, core):
        vals = {"b": core // 4, "j": core % 4, "jh": (core % 4) // 2, "jl": (core % 4) % 2}
        return self.c + sum(v * vals[n] for n, v in self.co.items())


VB, VJ, VJH, VJL = Lin(b=1), Lin(j=1), Lin(jh=1), Lin(jl=1)


class Res:
    def __init__(self, name):
        self.name = name
        self.w = None
        self.r = []


class Buf:
    def __init__(self, t, name):
        self.t = t
        self.name = name
        self.whole = Res(name)
        self.parts = {}

    def __getitem__(self, key):
        return self.t[key]

    def p(self, key):
        return (self, key)


class KB:
    NDMA = 24

    def __init__(self, nc, stack):
        self.nc = nc
        self.root = stack
        self.stack = stack
        self.eng = {"pe": nc.tensor, "act": nc.scalar, "dve": nc.vector, "pool": nc.gpsimd, "sp": nc.sync}
        self.sem = {e: stack.enter_context(nc.semaphore("c_" + e)) for e in self.eng}
        self.cnt = {e: 0 for e in self.eng}
        self.known = {e: {} for e in self.eng}
        self.dsem = [stack.enter_context(nc.semaphore("d%d" % i)) for i in range(self.NDMA)]
        self.dval = [0] * self.NDMA
        self.dnext = 0
        self.ccsem = stack.enter_context(nc.semaphore("ccsem"))
        self.ccval = 0
        self.out_tokens = []
        self.nbuf = 0
        self.V = None

    def sb(self, shape, dtype=F32, name=None):
        self.nbuf += 1
        name = "%s_%d" % (name or "sb", self.nbuf)
        t = self.stack.enter_context(self.nc.sbuf_tensor(name, list(shape), dtype))
        return Buf(t, name)

    def ps(self, shape, dtype=F32, name=None):
        self.nbuf += 1
        name = "%s_%d" % (name or "ps", self.nbuf)
        t = self.stack.enter_context(self.nc.psum_tensor(name, list(shape), dtype))
        return Buf(t, name)

    def dram(self, name, shape, dtype, kind=None):
        if kind is None:
            t = self.nc.dram_tensor(name, list(shape), dtype)
        else:
            t = self.nc.dram_tensor(name, list(shape), dtype, kind=kind)
        return Buf(t, name)

    def _states(self, r):
        if isinstance(r, Buf):
            return [r.whole] + list(r.parts.values()), r.whole
        b, k = r
        if k not in b.parts:
            b.parts[k] = Res("%s/%s" % (b.name, k))
        return [b.whole, b.parts[k]], b.parts[k]

    def _wait(self, e, tok):
        if tok is None:
            return
        kind, a, n = tok
        if kind == "eng":
            if a == "pe" and e == "pe":
                return
            key, sem = ("eng", a), self.sem[a]
        elif kind == "cc":
            key, sem = ("cc", 0), self.ccsem
        else:
            key, sem = ("dma", a), self.dsem[a]
        if self.known[e].get(key, 0) >= n:
            return
        self.known[e][key] = n
        self.eng[e].wait_ge(sem, n)

    def _deps(self, e, reads, writes):
        for r in reads:
            for s in self._states(r)[0]:
                self._wait(e, s.w)
        for w in writes:
            for s in self._states(w)[0]:
                self._wait(e, s.w)
                for t in s.r:
                    self._wait(e, t)

    def _commit(self, tok, reads, writes):
        for r in reads:
            self._states(r)[1].r.append(tok)
        for w in writes:
            sts, own = self._states(w)
            if isinstance(w, Buf):
                for s in sts:
                    s.w = tok
                    s.r = []
            else:
                own.w = tok
                own.r = []

    def op(self, e, fn, reads=(), writes=()):
        self._deps(e, reads, writes)
        ins = fn(self.eng[e])
        self.cnt[e] += 1
        ins.then_inc(self.sem[e], 1)
        tok = ("eng", e, self.cnt[e])
        self._commit(tok, reads, writes)
        return tok

    def _dma_issue(self, q, make, reads, writes, is_output):
        self._deps(q, reads, writes)
        s = self.dnext
        self.dnext = (self.dnext + 1) % self.NDMA
        if self.dval[s] > 0:
            self._wait(q, ("dma", s, self.dval[s]))
        ins = make()
        self.dval[s] += 16
        ins.then_inc(self.dsem[s], 16)
        tok = ("dma", s, self.dval[s])
        self._commit(tok, reads, writes)
        if is_output:
            self.out_tokens.append(tok)
        return tok

    def dma(self, q, out_ap, in_ap, reads=(), writes=(), is_output=False):
        return self._dma_issue(q, lambda: self.eng[q].dma_start(out=out_ap, in_=in_ap), reads, writes, is_output)

    def load_coords(self, meta_buf):
        self._deps("pool", [meta_buf], [])
        self.rcore = self.root.enter_context(self.nc.gpsimd.register("rcore"))
        self.rbh = self.root.enter_context(self.nc.gpsimd.register("rbh"))
        self.nc.gpsimd.reg_load(self.rcore, meta_buf.t[0:1, 0:1])
        self.nc.gpsimd.reg_load(self.rbh, meta_buf.t[0:1, 1:2])

    def dma_dyn(self, out_ap, src, off, pattern, reads=(), writes=(), slow=False):
        q = "pool"
        reads = list(reads) + [src]
        self._deps(q, reads, writes)
        s = self.dnext
        self.dnext = (self.dnext + 1) % self.NDMA
        if self.dval[s] > 0:
            self._wait(q, ("dma", s, self.dval[s]))
        if set(off.co) <= {"b", "jh"}:
            reg, cores = self.rbh, [(2 * b + jh, 4 * b + 2 * jh) for b in range(2) for jh in range(2)]
        else:
            reg, cores = self.rcore, [(c, c) for c in range(8)]
        g = self.nc.gpsimd
        for v, core in cores:
            with g.If_eq(reg, v):
                g.dma_start(out=out_ap, in_=bass.AP(src.t, off.at(core), pattern), **({"allow_slow_non_contiguous": True} if slow else {})).then_inc(self.dsem[s], 16)
        g.end_ifs()
        self.dval[s] += 16
        tok = ("dma", s, self.dval[s])
        self._commit(tok, reads, writes)
        return tok

    def all_gather(self, src, dst):
        self._deps("pool", [src], [dst])
        ins = self.nc.gpsimd.collective_compute("AllGather", ALU.bypass, replica_groups=[list(range(8))],
                                                ins=[src.t[:, :]], outs=[dst.t[:, :]])
        self.ccval += 1
        ins.then_inc(self.ccsem, 1)
        tok = ("cc", 0, self.ccval)
        self._commit(tok, [src], [dst])
        return tok

    def barrier(self):
        for e in self.eng:
            for f in self.eng:
                if f != e and self.cnt[f] > 0:
                    self._wait(e, ("eng", f, self.cnt[f]))
            for s in range(self.NDMA):
                if self.dval[s] > 0:
                    self._wait(e, ("dma", s, self.dval[s]))
            if self.ccval > 0:
                self._wait(e, ("cc", 0, self.ccval))

    class _Phase:
        def __init__(self, k):
            self.k = k

        def __enter__(self):
            self.st = ExitStack()
            self.st.__enter__()
            self.prev = self.k.stack
            self.k.stack = self.st
            return self

        def __exit__(self, *a):
            if a[0] is None:
                self.k.barrier()
            self.k.stack = self.prev
            return self.st.__exit__(*a)

    def phase(self):
        return KB._Phase(self)

    def finish(self, e="sp"):
        self.barrier()
D = 1024
SEQ = 8192
NB = 2
CTX = 256
TL = 2048
TCX = 64
TC = TL + TCX
TS = CTX + SEQ
EPS = 1e-6
D_FF = 2816
NFC = D_FF // 128
NEG = -100.0
NCHK = TS // 128
NCH = TS // 64
YW = TS + 4

O_AQ, O_AK, O_AV = 0, 256, 384
O_BQ, O_BK, O_BV = 512, 768, 896
O_CQ, O_CK, O_CV, O_CO, O_CG = 1024, 1280, 1536, 1792, 2048
O_DQ, O_DK, O_DV = 2064, 2320, 2576


def _swap_idx():
    d = np.arange(64)
    i = d % 32
    return np.where(i < 16, d + 16, d - 16)


SW64 = _swap_idx()
SW128 = np.concatenate([SW64, SW64 + 64])

P_TILES = [
    ("aq0", O_AQ, "ropeA_q", ("b", 0)), ("aq1", O_AQ + 128, "ropeA_q", ("b", 1)), ("ak", O_AK, "ropeA_k", ("b", 2)),
    ("bq0", O_BQ, "ropeB_q", ("b", 3)), ("bq1", O_BQ + 128, "ropeB_q", ("b", 4)), ("bk", O_BK, "ropeB_k", ("b", 5)),
    ("dq0", O_DQ, "plain_q", ("b", 6)), ("dq1", O_DQ + 128, "plain_q", ("b", 7)),
    ("dk0", O_DK, "plain", ("b", 8)), ("dk1", O_DK + 128, "plain", ("b", 9)),
    ("av", O_AV, "tok", ("bv", 0)), ("bv", O_BV, "tok", ("bv", 1)), ("dv0", O_DV, "tok", ("bv", 2)), ("dv1", O_DV + 128, "tok", ("bv", 3)),
    ("cq0", O_CQ, "plain_q", ("c", 0)), ("cq1", O_CQ + 128, "plain_q", ("c", 1)),
    ("ck0", O_CK, "plain", ("c", 2)), ("ck1", O_CK + 128, "plain", ("c", 3)),
    ("co0", O_CO, "plain", ("c", 4)), ("co1", O_CO + 128, "plain", ("c", 5)),
    ("ck0t", O_CK, "tok", ("ct", 0)), ("ck1t", O_CK + 128, "tok", ("ct", 1)),
    ("cv0t", O_CV, "tok", ("ct", 2)), ("cv1t", O_CV + 128, "tok", ("ct", 3)),
]
B_FM = 10 * 128
B_ROWS = B_FM + 512
C_FM = 6 * 128
C_G = C_FM
C_TOK = C_FM + 16
C_ROWS = C_TOK + 512
Y_ROWS = 384
Y_A, Y_B, Y_HF, Y_HB, Y_D, Y_O = 0, 64, 128, 192, 256, 320


def p_weight_cols():
    cols = []
    offs = {}
    for name, off, kind, _ in P_TILES:
        offs[name] = len(cols)
        cols.extend(range(off, off + 128))
        if kind.startswith("rope"):
            cols.extend((off + SW128).tolist())
    offs["cg"] = len(cols)
    cols.extend(range(O_CG, O_CG + 16))
    return np.array(cols), offs


P_COLS, P_OFFS = p_weight_cols()
NWP = len(P_COLS)
TBLOCKS = [(0, 512), (512, 512), (1024, 512), (1536, 512), (2048, 64)]
def phase_P(k, xsrc, xcol0, W, S1b, S1c):
    nc = k.nc
    with k.phase():
        ones = k.sb([128, 128], F32, "ones")
        bones = k.sb([128, 128], F32, "bones")
        k.op("pool", lambda e: e.memset(ones[:], 1.0), writes=[ones])
        k.op("pool", lambda e: e.memset(bones[:], 0.0), writes=[bones])
        k.op("pool", lambda e: e.memset(bones[0:64, 0:64], 1.0), writes=[bones])
        k.op("pool", lambda e: e.memset(bones[64:128, 64:128], 1.0), writes=[bones])
        bms = k.sb([128, 16], F32, "bms"); cvs = k.sb([128, 8, 2], F32, "cvs")
        g1s = k.sb([128, 8], F32, "g1s"); gns = k.sb([128, 4], F32, "gns")
        coss = k.sb([128, TC], F32, "coss"); sins = k.sb([128, TC], F32, "sins")
        k.dma("sp", bms[:], W["bm1"][:], writes=[bms]); k.dma("sp", cvs[:], W["cv"][:], writes=[cvs])
        k.dma("sp", g1s[:], W["g1"][:], writes=[g1s]); k.dma("sp", gns[:], W["gn"][:], writes=[gns])
        k.dma("sp", coss[:], W["cos"][:], writes=[coss]); k.dma("sp", sins[:], W["sin"][:], writes=[sins])
        k.op("dve", lambda e: e.tensor_scalar(gns[:, 0:2], gns[:, 0:2], 0.125, None, op0=ALU.mult), reads=[gns], writes=[gns])
        sc = k.sb([128, 8, 2], F32, "silu_c")
        k.op("act", lambda e: e.activation(out=sc[:], in_=cvs[:], func=AF.Silu), reads=[cvs], writes=[sc])
        mod = k.sb([128, 16, 2], F32, "mod")
        pmod = k.ps([128, 512], F32, "pmod")
        wmb = [k.sb([128, 8, 128], F32, "wmb") for i in range(2)]
        wm_v = W["wm1"].t.rearrange("(kc p) c -> p kc c", p=128)
        for j in range(16):
            wb_ = wmb[j % 2]
            k.dma("sp", wb_[:], wm_v[:, :, j * 128:(j + 1) * 128], writes=[wb_])
            for kc in range(8):
                k.op("pe", lambda e, kc=kc, wb_=wb_: e.matmul(pmod[:, 0:2], lhsT=wb_[:, kc, :], rhs=sc[:, kc, :], start=(kc == 0), stop=(kc == 7)),
                     reads=[wb_, sc], writes=[pmod])
            k.op("dve", lambda e, j=j: e.tensor_scalar(mod[:, j, :], pmod[:, 0:2], bms[:, j:j + 1], None, op0=ALU.add), reads=[pmod, bms], writes=[mod.p(j)])
        Asc = k.sb([128, 8, 2], F32, "Asc")
        k.op("dve", lambda e: e.tensor_scalar(Asc[:], mod[:, 8:16, :], 1.0, None, op0=ALU.add), reads=[mod], writes=[Asc])
        for w_ in range(2):
            k.op("dve", lambda e, w_=w_: e.tensor_tensor(out=Asc[:, :, w_], in0=Asc[:, :, w_], in1=g1s[:], op=ALU.mult), reads=[Asc, g1s], writes=[Asc])

        hT = k.sb([128, 8, TC], BF16, "hT")
        xb = [k.sb([128, 8, 512], F32, "xb") for i in range(2)]
        sq = k.sb([128, 8, 512], F32, "sq")
        pss = k.ps([128, 512], F32, "pss")
        rstd = k.sb([128, 512], F32, "rstd")
        xn = k.sb([128, 512], F32, "xn")
        xw = xsrc.t.shape[1]
        xT_v = xsrc.t.rearrange("(kc p) t -> p kc t", p=128)
        for bi, (t0, tn) in enumerate(TBLOCKS):
            xbb = xb[bi % 2]
            which = 1 if bi == 4 else 0
            s0 = (xw - TCX) if bi == 4 else (xcol0 + t0)
            k.dma("sp", xbb[:, :, 0:tn], xT_v[:, :, s0:s0 + tn], reads=[xsrc], writes=[xbb])
            k.op("act", lambda e, xbb=xbb, tn=tn: e.activation(out=sq[:, :, 0:tn], in_=xbb[:, :, 0:tn], func=AF.Square), reads=[xbb], writes=[sq])
            for kc in range(8):
                k.op("pe", lambda e, kc=kc, tn=tn: e.matmul(pss[:, 0:tn], lhsT=ones[:], rhs=sq[:, kc, 0:tn], start=(kc == 0), stop=(kc == 7)), reads=[ones, sq], writes=[pss])
            k.op("act", lambda e, tn=tn: e.activation(out=rstd[:, 0:tn], in_=pss[:, 0:tn], func=AF.Sqrt, scale=1.0 / D, bias=EPS), reads=[pss], writes=[rstd])
            k.op("dve", lambda e, tn=tn: e.reciprocal(rstd[:, 0:tn], rstd[:, 0:tn]), reads=[rstd], writes=[rstd])
            for kc in range(8):
                k.op("dve", lambda e, kc=kc, tn=tn, xbb=xbb: e.tensor_tensor(out=xn[:, 0:tn], in0=xbb[:, kc, 0:tn], in1=rstd[:, 0:tn], op=ALU.mult), reads=[xbb, rstd], writes=[xn])
                k.op("act", lambda e, kc=kc, tn=tn, t0=t0, which=which: e.activation(out=hT[:, kc, t0:t0 + tn], in_=xn[:, 0:tn], func=AF.Identity,
                                                                                 scale=Asc[:, kc, which:which + 1], bias=mod[:, kc, which:which + 1]),
                     reads=[xn, Asc, mod], writes=[hT.p(bi)])

        wst = [k.sb([128, 8, 256], F32, "wst") for i in range(2)]
        wbf = [k.sb([128, 8, 256], BF16, "wbf") for i in range(2)]
        px = [k.ps([128, 512], F32, "px") for i in range(2)]
        pxs = [k.ps([128, 512], F32, "pxs") for i in range(2)]
        pn = k.ps([128, 512], F32, "pn")
        ob = [k.sb([128, 512], BF16, "ob") for i in range(2)]
        of = [k.sb([128, 512], F32, "of") for i in range(2)]
        e1 = k.sb([128, 512], F32, "e1"); e2 = k.sb([128, 512], F32, "e2"); e3 = k.sb([128, 512], F32, "e3")
        rs2 = k.sb([128, 512], F32, "rs2")
        wp_v = W["wp"].t.rearrange("(kc p) c -> p kc c", p=128)
        it = 0
        for ti, (name, off, kind, (okind, orow)) in enumerate(P_TILES + [("cg", O_CG, "gates", ("g", 0))]):
            rope = kind.startswith("rope")
            ncol = 256 if rope else (16 if kind == "gates" else 128)
            c0 = P_OFFS[name]
            ws_, wb_ = wst[ti % 2], wbf[ti % 2]
            k.dma("sp", ws_[:, :, 0:ncol], wp_v[:, :, c0:c0 + ncol], writes=[ws_])
            k.op("pool", lambda e, ws_=ws_, wb_=wb_, ncol=ncol: e.tensor_copy(wb_[:, :, 0:ncol], ws_[:, :, 0:ncol]), reads=[ws_], writes=[wb_])
            M = 16 if kind == "gates" else 128
            if kind == "tok":
                dbuf = S1b if okind == "bv" else S1c
                base = (B_FM if okind == "bv" else C_TOK) * TC + orow * TC * 128
                for g0 in range(0, 17, 4):
                    subs = list(range(g0, min(g0 + 4, 17)))
                    pX = px[it % 2]
                    o_ = (ob if okind == "bv" else of)[it % 2]
                    for si, s in enumerate(subs):
                        tn = 128 if s < 16 else 64
                        for kc in range(8):
                            k.op("pe", lambda e, kc=kc, pX=pX, wb_=wb_, si=si, s=s, tn=tn: e.matmul(
                                pX[0:tn, si * 128:(si + 1) * 128], lhsT=hT[:, kc, s * 128:s * 128 + tn], rhs=wb_[:, kc, 0:128], start=(kc == 0), stop=(kc == 7)),
                                reads=[wb_, hT.p(min(s // 4, 4))], writes=[pX])
                    nfull = len([s for s in subs if s < 16])
                    if nfull:
                        k.op("act", lambda e, pX=pX, o_=o_, nfull=nfull: e.activation(out=o_[:, 0:nfull * 128], in_=pX[:, 0:nfull * 128], func=AF.Identity), reads=[pX], writes=[o_])
                        dst = bass.AP(dbuf.t, base + g0 * 128 * 128, [[128, 128], [128 * 128, nfull], [1, 128]])
                        k.dma("pool", dst, o_[:, 0:nfull * 128].rearrange("p (s c) -> p s c", c=128), reads=[o_], writes=[dbuf.p((ti, g0))])
                    if 16 in subs:
                        si = subs.index(16)
                        o2 = (ob if okind == "bv" else of)[(it + 1) % 2]
                        k.op("act", lambda e, pX=pX, o2=o2, si=si: e.activation(out=o2[0:64, 0:128], in_=pX[0:64, si * 128:(si + 1) * 128], func=AF.Identity), reads=[pX], writes=[o2])
                        dst = bass.AP(dbuf.t, base + 2048 * 128, [[128, 64], [1, 128]])
                        k.dma("pool", dst, o2[0:64, 0:128], reads=[o2], writes=[dbuf.p((ti, 99))])
                    it += 1
                continue
            for bi, (t0, tn) in enumerate(TBLOCKS):
                pX, pXs = px[it % 2], pxs[it % 2]
                for kc in range(8):
                    k.op("pe", lambda e, kc=kc, pX=pX, wb_=wb_, M=M, t0=t0, tn=tn: e.matmul(pX[0:M, 0:tn], lhsT=wb_[:, kc, 0:M], rhs=hT[:, kc, t0:t0 + tn], start=(kc == 0), stop=(kc == 7)),
                         reads=[wb_, hT.p(bi)], writes=[pX])
                if rope:
                    for kc in range(8):
                        k.op("pe", lambda e, kc=kc, pXs=pXs, wb_=wb_, t0=t0, tn=tn: e.matmul(pXs[:, 0:tn], lhsT=wb_[:, kc, 128:256], rhs=hT[:, kc, t0:t0 + tn], start=(kc == 0), stop=(kc == 7)),
                             reads=[wb_, hT.p(bi)], writes=[pXs])
                if okind == "b":
                    o_ = ob[it % 2]; dbuf = S1b; r0 = orow * 128
                elif okind == "c":
                    o_ = of[it % 2]; dbuf = S1c; r0 = orow * 128
                else:
                    o_ = of[it % 2]; dbuf = S1c; r0 = C_G
                dst = dbuf.t[r0:r0 + M, t0:t0 + tn]
                if rope:
                    isA = kind.startswith("ropeA")
                    isq = kind.endswith("_q")
                    if isA:
                        g0 = 0 if isq else 2
                        s1, s2 = gns[:, g0:g0 + 1], gns[:, g0 + 1:g0 + 2]
                    else:
                        s1 = s2 = (0.125 if isq else 1.0)
                    k.op("act", lambda e, pX=pX, tn=tn, s1=s1: e.activation(out=e1[:, 0:tn], in_=pX[:, 0:tn], func=AF.Identity, scale=s1), reads=[pX, gns], writes=[e1])
                    k.op("act", lambda e, pXs=pXs, tn=tn, s2=s2: e.activation(out=e2[:, 0:tn], in_=pXs[:, 0:tn], func=AF.Identity, scale=s2), reads=[pXs, gns], writes=[e2])
                    if isA:
                        k.op("act", lambda e, pX=pX, tn=tn: e.activation(out=e3[:, 0:tn], in_=pX[:, 0:tn], func=AF.Square), reads=[pX], writes=[e3])
                        k.op("pe", lambda e, tn=tn: e.matmul(pn[:, 0:tn], lhsT=bones[:], rhs=e3[:, 0:tn], start=True, stop=True), reads=[bones, e3], writes=[pn])
                        k.op("act", lambda e, tn=tn: e.activation(out=rs2[:, 0:tn], in_=pn[:, 0:tn], func=AF.Sqrt, scale=1.0 / 64, bias=EPS), reads=[pn], writes=[rs2])
                        k.op("dve", lambda e, tn=tn: e.reciprocal(rs2[:, 0:tn], rs2[:, 0:tn]), reads=[rs2], writes=[rs2])
                    k.op("dve", lambda e, tn=tn, t0=t0: e.tensor_tensor(out=e1[:, 0:tn], in0=e1[:, 0:tn], in1=coss[:, t0:t0 + tn], op=ALU.mult), reads=[e1, coss], writes=[e1])
                    k.op("dve", lambda e, tn=tn, t0=t0: e.tensor_tensor(out=e2[:, 0:tn], in0=e2[:, 0:tn], in1=sins[:, t0:t0 + tn], op=ALU.mult), reads=[e2, sins], writes=[e2])
                    if isA:
                        k.op("dve", lambda e, tn=tn: e.tensor_tensor(out=e1[:, 0:tn], in0=e1[:, 0:tn], in1=e2[:, 0:tn], op=ALU.add), reads=[e1, e2], writes=[e1])
                        k.op("dve", lambda e, tn=tn, o_=o_: e.tensor_tensor(out=o_[:, 0:tn], in0=e1[:, 0:tn], in1=rs2[:, 0:tn], op=ALU.mult), reads=[e1, rs2], writes=[o_])
                    else:
                        k.op("dve", lambda e, tn=tn, o_=o_: e.tensor_tensor(out=o_[:, 0:tn], in0=e1[:, 0:tn], in1=e2[:, 0:tn], op=ALU.add), reads=[e1, e2], writes=[o_])
                else:
                    scl = 0.125 if kind == "plain_q" else 1.0
                    k.op("act", lambda e, pX=pX, tn=tn, o_=o_, M=M, scl=scl: e.activation(out=o_[0:M, 0:tn], in_=pX[0:M, 0:tn], func=AF.Identity, scale=scl), reads=[pX], writes=[o_])
                k.dma("pool", dst, o_[0:M, 0:tn], reads=[o_], writes=[dbuf.p((ti, bi))])
                it += 1
def attn_schedules(need_ctx):
    sA, sB, sD = [], [], []
    if need_ctx:
        for s in (sA, sB, sD):
            s.append((0, 256, [(0, None), (1, None)]))
    for m in range(16):
        q0 = CTX + 512 * m
        sA.append((q0, 512, [(c, None) for c in range(NCHK)]))
        lb = []
        for r in range(6):
            n = 4 * m - 1 + r
            if 0 <= n < 64:
                lb.append((n + 2, r))
        sB.append((q0, 512, lb + [(0, None), (1, None)]))
        cls = 0 if m == 0 else (2 if m == 15 else 1)
        ld = []
        for o in range(8):
            n = 4 * m - 2 + o
            if 0 <= n < 64:
                ld.append((n + 2, 6 + cls * 8 + o))
        sD.append((q0, 512, ld + [(0, None), (1, None)]))
    return sA, sB, sD


def g1b_off(q, row, col):
    return (VB * 4 + q) * (B_ROWS * TC) + row * TC + col


def g1b_voff(q, vt, c0, tok0):
    return (VB * 4 + q) * (B_ROWS * TC) + B_FM * TC + (vt * TC + tok0) * 128 + c0


MA_SPEC = {
    "A": dict(qrow=VJ * 64, krow=VJH * 64 + 256, vt=Lin(0), vc=VJH * 64, yrow=Y_A),
    "B": dict(qrow=VJ * 64 + 384, krow=VJH * 64 + 640, vt=Lin(1), vc=VJH * 64, yrow=Y_B),
    "D": dict(qrow=VJ * 64 + 768, krow=VJ * 64 + 1024, vt=VJH + 2, vc=VJL * 64, yrow=Y_D),
}


NBIAS_T = 30


def phase_MA(k, need_ctx, W, G1b, S2):
    with k.phase():
        QT = k.sb([64, TS], BF16, "QT"); KT = k.sb([64, TS], BF16, "KT")
        VA = k.sb([128, NCHK, 65], BF16, "VA")
        sinks = k.sb([128, 1], F32, "sinks"); esink = k.sb([128, 1], F32, "esink")
        sel = k.sb([65, 64], F32, "sel")
        k.op("pool", lambda e: e.memset(sel[:], 0.0), writes=[sel])
        k.op("pool", lambda e: e.memset(sel[64:65, :], 1.0), writes=[sel])
        k.dma("sp", sinks[:], W["sink"][:], writes=[sinks])
        k.op("act", lambda e: e.activation(out=esink[:], in_=sinks[:], func=AF.Exp), reads=[sinks], writes=[esink])
        pS = [k.ps([128, 512], F32, "pS") for i in range(3)]
        pO = [k.ps([128, 512], F32, "pO") for i in range(2)]
        pD = k.ps([128, 512], F32, "pD")
        PT = [k.sb([128, 512], BF16, "PT") for i in range(4)]
        ball = k.sb([128, NBIAS_T, 512], F32, "ball")
        bias_v = W["bias"].t.rearrange("t p c -> p t c")
        for t0 in range(0, NBIAS_T, 5):
            k.dma("sp", ball[:, t0:t0 + 5, :], bias_v[:, t0:t0 + 5, :], writes=[ball.p(t0 // 5)])
        sbuf = [k.sb([128, 512], F32, "sbf") for i in range(2)]
        ot = [k.sb([65, 512], F32, "ot") for i in range(2)]
        rr = k.sb([64, 512], F32, "rr")
        yo = [k.sb([64, 512], F32, "yo") for i in range(2)]
        scheds = dict(zip("ABD", attn_schedules(need_ctx)))
        ib = [0]
        iq = 0
        for X in "ABD":
            sp_ = MA_SPEC[X]
            for q in range(4):
                k.dma_dyn(QT[:, CTX + TL * q:CTX + TL * (q + 1)], G1b, g1b_off(q, sp_["qrow"], 0), [[TC, 64], [1, TL]], writes=[QT.p(("l", q))])
                k.dma_dyn(QT[:, TCX * q:TCX * (q + 1)], G1b, g1b_off(q, sp_["qrow"], TL), [[TC, 64], [1, TCX]], writes=[QT.p(("c", q))])
                k.dma_dyn(KT[:, CTX + TL * q:CTX + TL * (q + 1)], G1b, g1b_off(q, sp_["krow"], 0), [[TC, 64], [1, TL]], writes=[KT.p(("l", q))])
                k.dma_dyn(KT[:, TCX * q:TCX * (q + 1)], G1b, g1b_off(q, sp_["krow"], TL), [[TC, 64], [1, TCX]], writes=[KT.p(("c", q))])
                for i0 in range(0, 16, 4):
                    k.dma_dyn(VA[:, 2 + 16 * q + i0:2 + 16 * q + i0 + 4, 0:64], G1b, g1b_voff(q, sp_["vt"], sp_["vc"], i0 * 128), [[128, 128], [128 * 128, 4], [1, 64]], writes=[VA.p(("l", q, i0))])
                k.dma_dyn(VA[(q % 2) * 64:(q % 2) * 64 + 64, q // 2, 0:64], G1b, g1b_voff(q, sp_["vt"], sp_["vc"], TL), [[128, 64], [1, 64]], writes=[VA.p(("c", q))])
            k.op("pool", lambda e: e.memset(VA[:, :, 64:65], 1.0), writes=[VA.p("ones")])
            tiles = []
            for qi, (q0, nq, chunks) in enumerate(scheds[X]):
                for ci, (c, bidx) in enumerate(chunks):
                    tiles.append((qi, q0, nq, ci, len(chunks), c, bidx))
            LA = 2

            def emit_S(ti):
                qi, q0, nq, ci, nchk, c, bidx = tiles[ti]
                ps = pS[ti % 3]
                pt = PT[ti % 4]
                k.op("pe", lambda e: e.matmul(ps[:, 0:nq], lhsT=KT[:, c * 128:(c + 1) * 128], rhs=QT[:, q0:q0 + nq], start=True, stop=True), reads=[KT, QT], writes=[ps])
                if bidx is not None:
                    sbt = sbuf[ti % 2]
                    k.op("dve", lambda e: e.tensor_tensor(out=sbt[:, 0:nq], in0=ps[:, 0:nq], in1=ball[:, bidx, 0:nq], op=ALU.add), reads=[ps, ball.p(bidx // 5)], writes=[sbt])
                    k.op("act", lambda e: e.activation(out=pt[:, 0:nq], in_=sbt[:, 0:nq], func=AF.Exp), reads=[sbt], writes=[pt])
                else:
                    k.op("act", lambda e: e.activation(out=pt[:, 0:nq], in_=ps[:, 0:nq], func=AF.Exp), reads=[ps], writes=[pt])
            for ti in range(min(LA, len(tiles))):
                emit_S(ti)
            for ti, (qi, q0, nq, ci, nchk, c, bidx) in enumerate(tiles):
                if ti + LA < len(tiles):
                    emit_S(ti + LA)
                po = pO[qi % 2]
                pt = PT[ti % 4]
                k.op("pe", lambda e, po=po, pt=pt, c=c, ci=ci, nchk=nchk, nq=nq: e.matmul(po[0:65, 0:nq], lhsT=VA[:, c, :], rhs=pt[:, 0:nq], start=(ci == 0), stop=(ci == nchk - 1)),
                     reads=[pt, VA], writes=[po])
                if ci != nchk - 1:
                    continue
                otb = ot[iq % 2]; yob = yo[iq % 2]; iq += 1
                k.op("act", lambda e, po=po, otb=otb, nq=nq: e.activation(out=otb[:, 0:nq], in_=po[0:65, 0:nq], func=AF.Identity), reads=[po], writes=[otb])
                k.op("pe", lambda e, otb=otb, nq=nq: e.matmul(pD[0:64, 0:nq], lhsT=sel[:, :], rhs=otb[:, 0:nq], start=True, stop=True), reads=[sel, otb], writes=[pD])
                if X == "B":
                    k.op("dve", lambda e, nq=nq: e.tensor_scalar(rr[:, 0:nq], pD[0:64, 0:nq], esink[0:64, 0:1], None, op0=ALU.add), reads=[pD, esink], writes=[rr])
                    k.op("dve", lambda e, nq=nq: e.reciprocal(rr[:, 0:nq], rr[:, 0:nq]), reads=[rr], writes=[rr])
                else:
                    k.op("dve", lambda e, nq=nq: e.reciprocal(rr[:, 0:nq], pD[0:64, 0:nq]), reads=[pD], writes=[rr])
                k.op("dve", lambda e, otb=otb, yob=yob, nq=nq: e.tensor_tensor(out=yob[:, 0:nq], in0=otb[0:64, 0:nq], in1=rr[:, 0:nq], op=ALU.mult), reads=[otb, rr], writes=[yob])
                r0 = sp_["yrow"]
                k.dma("sp", S2.t[r0:r0 + 64, 2 + q0:2 + q0 + nq], yob[:, 0:nq], reads=[yob], writes=[S2.p((X, q0))])
def g1c_off(q, row, col):
    return (VB * 4 + q) * (C_ROWS * TC) + row * TC + col


def g1c_toff(q, vt, tok0):
    return (VB * 4 + q) * (C_ROWS * TC) + C_TOK * TC + (vt * TC + tok0) * 128 + VJL * 64


MC_SEGS = [("c", 0, 4, 0), ("l", 0, 32, 4), ("l", 1, 32, 36), ("l", 2, 32, 68), ("l", 3, 32, 100)]
SEGC = 32


def phase_MC(k, W, G1c, S2):
    with k.phase():
        tri2 = k.sb([64, 2, 64], F32, "tri2"); ones = k.sb([64, 64], F32, "ones64")
        gb = k.sb([64, 4], F32, "gb"); nb = k.sb([64, 4], F32, "nb")
        sel = k.sb([65, 64], F32, "sel")
        k.op("pool", lambda e: e.memset(sel[:], 0.0), writes=[sel])
        k.op("pool", lambda e: e.memset(sel[64:65, :], 1.0), writes=[sel])
        k.dma("sp", tri2[:], W["tri"][:], writes=[tri2]); k.dma("sp", gb[:], W["gb"][:], writes=[gb])
        k.op("pool", lambda e: e.memset(ones[:], 1.0), writes=[ones])
        k.op("dve", lambda e: e.tensor_scalar(nb[:], gb[:], -1.0, None, op0=ALU.mult), reads=[gb], writes=[nb])
        for q in range(4):
            k.dma_dyn(S2.t[Y_O:Y_O + 64, 2 + CTX + TL * q:2 + CTX + TL * (q + 1)], G1c, g1c_off(q, VJ * 64 + 512, 0), [[TC, 64], [1, TL]], writes=[S2.p(("o", q))])
            k.dma_dyn(S2.t[Y_O:Y_O + 64, 2 + TCX * q:2 + TCX * (q + 1)], G1c, g1c_off(q, VJ * 64 + 512, TL), [[TC, 64], [1, TCX]], writes=[S2.p(("oc", q))])
        gates = k.sb([64, 4, NCH], F32, "gates")
        ident = k.sb([64, 64], F32, "ident")
        k.op("dve", lambda e: e.tensor_tensor(out=ident[:], in0=tri2[:, 0, :], in1=tri2[:, 1, :], op=ALU.mult), reads=[tri2], writes=[ident])
        graw = [k.sb([32, 64], F32, "graw") for i in range(2)]
        gctx = [k.sb([4, 64], F32, "gctx") for i in range(2)]
        pg = [k.ps([128, 512], F32, "pg") for i in range(1)]
        ig = 0
        for kind in range(4):
            for q in range(4):
                gr = graw[ig % 2]; ig += 1
                k.dma_dyn(gr[:], G1c, g1c_off(q, VJ + (C_G + kind * 4), 0), [[64, 32], [1, 64]], writes=[gr])
                k.op("pe", lambda e, gr=gr: e.matmul(pg[0][0:64, 0:32], lhsT=gr[:], rhs=ident[0:32, 0:32], start=True, stop=True), reads=[gr, ident], writes=[pg[0]])
                k.op("act", lambda e, kind=kind, q=q: e.activation(out=gates[:, kind, 4 + 32 * q:4 + 32 * (q + 1)], in_=pg[0][0:64, 0:32], func=AF.Identity), reads=[pg[0]], writes=[gates.p((kind, q))])
            gc = gctx[kind % 2]
            for q in range(4):
                k.dma_dyn(gc[q:q + 1, :], G1c, g1c_off(q, VJ + (C_G + kind * 4), TL), [[64, 1], [1, 64]], writes=[gc.p(q)])
            k.op("pe", lambda e, gc=gc: e.matmul(pg[0][0:64, 0:4], lhsT=gc[:], rhs=ident[0:4, 0:4], start=True, stop=True), reads=[gc, ident], writes=[pg[0]])
            k.op("act", lambda e, kind=kind: e.activation(out=gates[:, kind, 0:4], in_=pg[0][0:64, 0:4], func=AF.Identity), reads=[pg[0]], writes=[gates.p((kind, "c"))])
        li = k.sb([64, NCH], F32, "li"); sp = k.sb([64, NCH], F32, "sp"); bcol = k.sb([64, NCH], F32, "bcol")
        W_ = SEGC * 64
        qT = k.sb([64, W_], F32, "qT"); kT = k.sb([64, W_], F32, "kT"); ktok = k.sb([64, W_], F32, "ktok")
        vaug = k.sb([64, SEGC * 65], F32, "vaug")
        Z = k.sb([64, W_], F32, "Z"); Ab = k.sb([64, W_], F32, "Ab"); Wb = k.sb([64, W_], F32, "Wb")
        qw = k.sb([64, W_], F32, "qw"); kw = k.sb([64, W_], F32, "kw"); Dm = k.sb([64, W_], F32, "Dm"); SD = k.sb([64, W_], F32, "SD")
        U = k.sb([64, SEGC * 65], F32, "U"); CB = k.sb([64, SEGC * 65], F32, "CB"); carry = k.sb([64, 65], F32, "carry")
        wend = k.sb([64, SEGC], F32, "wend")
        ot = [k.sb([65, 512], F32, "ot") for i in range(2)]
        rr = k.sb([64, 512], F32, "rr"); hh = [k.sb([64, 512], F32, "hh") for i in range(2)]
        psb = [k.ps([128, 512], F32, "psb") for i in range(6)]
        pi = [0]

        def nextps():
            p = psb[pi[0] % 6]; pi[0] += 1
            return p
        vaug3 = vaug.t.rearrange("p (c e) -> p c e", e=65)
        ktok3 = ktok.t.rearrange("p (c e) -> p c e", e=64)
        Ab3 = Ab.t.rearrange("p (c t) -> p c t", t=64)
        k.op("pool", lambda e: e.memset(vaug3[:, :, 64:65], 1.0), writes=[vaug.p("ones")])
        alt = [0]

        def ve():
            alt[0] += 1
            return "dve" if alt[0] % 2 else "pool"
        io = 0
        for di in range(2):
            tri = tri2.t[:, di, :]
            fc = 63 if di == 0 else 0
            gi = gates.t[:, 2 * di, :]; gf = gates.t[:, 2 * di + 1, :]
            yrow = Y_HF if di == 0 else Y_HB
            k.op("dve", lambda e: e.tensor_scalar(li[:], gi, gb[:, 2 * di:2 * di + 1], None, op0=ALU.add), reads=[gates, gb], writes=[li])
            k.op("act", lambda e: e.activation(out=sp[:], in_=gf, func=AF.Exp, scale=-1.0, bias=nb[:, 2 * di + 1:2 * di + 2]), reads=[gates, nb], writes=[sp])
            k.op("act", lambda e: e.activation(out=sp[:], in_=sp[:], func=AF.Ln, scale=1.0, bias=1.0), reads=[sp], writes=[sp])
            pA = nextps()
            k.op("pe", lambda e: e.matmul(pA[0:64, 0:NCH], lhsT=tri, rhs=sp[:], start=True, stop=True), reads=[tri2, sp], writes=[pA])
            k.op("dve", lambda e: e.tensor_tensor(out=bcol[:], in0=pA[0:64, 0:NCH], in1=li[:], op=ALU.add), reads=[pA, li], writes=[bcol])
            k.op("dve", lambda e: e.memset(carry[:], 0.0), writes=[carry])
            segs = MC_SEGS if di == 0 else [MC_SEGS[0]] + MC_SEGS[:0:-1]
            for (skind, sq_, n, c0) in segs:
                Wn = n * 64
                if skind == "l":
                    q = sq_
                    k.dma_dyn(qT[:, 0:Wn], G1c, g1c_off(q, VJ * 64, 0), [[TC, 64], [1, TL]], writes=[qT])
                    k.dma_dyn(kT[:, 0:Wn], G1c, g1c_off(q, VJ * 64 + 256, 0), [[TC, 64], [1, TL]], writes=[kT])
                    for i0 in range(0, n, 8):
                        k.dma_dyn(ktok3[:, i0:i0 + 8, :], G1c, g1c_toff(q, VJH, i0 * 64), [[128, 64], [64 * 128, 8], [1, 64]], writes=[ktok.p(i0)])
                        k.dma_dyn(vaug3[:, i0:i0 + 8, 0:64], G1c, g1c_toff(q, VJH + 2, i0 * 64), [[128, 64], [64 * 128, 8], [1, 64]], writes=[vaug.p(("v", i0))])
                else:
                    for q in range(4):
                        k.dma_dyn(qT[:, q * 64:(q + 1) * 64], G1c, g1c_off(q, VJ * 64, TL), [[TC, 64], [1, 64]], writes=[qT.p(q)])
                        k.dma_dyn(kT[:, q * 64:(q + 1) * 64], G1c, g1c_off(q, VJ * 64 + 256, TL), [[TC, 64], [1, 64]], writes=[kT.p(q)])
                        k.dma_dyn(ktok3[:, q, :], G1c, g1c_toff(q, VJH, TL), [[128, 64], [1, 64]], writes=[ktok.p(q)])
                        k.dma_dyn(vaug3[:, q, 0:64], G1c, g1c_toff(q, VJH + 2, TL), [[128, 64], [1, 64]], writes=[vaug.p(("v", q))])
                for cc in range(n):
                    k.op(ve(), lambda e, cc=cc: e.tensor_scalar(Z[:, cc * 64:(cc + 1) * 64], tri, sp[:, c0 + cc:c0 + cc + 1], None, op0=ALU.mult), reads=[tri2, sp], writes=[Z.p(cc // 8)])
                for g0 in range(0, n, 8):
                    m = min(8, n - g0)
                    pz = nextps()
                    k.op("pe", lambda e, pz=pz, g0=g0, m=m: e.matmul(pz[0:64, 0:m * 64], lhsT=ones[:], rhs=Z[:, g0 * 64:(g0 + m) * 64], start=True, stop=True), reads=[ones, Z.p(g0 // 8)], writes=[pz])
                    k.op("act", lambda e, pz=pz, g0=g0, m=m: e.activation(out=Ab[:, g0 * 64:(g0 + m) * 64], in_=pz[0:64, 0:m * 64], func=AF.Identity, scale=-1.0), reads=[pz], writes=[Ab.p(g0 // 8)])
                k.op("act", lambda e: e.activation(out=Wb[:, 0:Wn], in_=Ab[:, 0:Wn], func=AF.Exp), reads=[Ab], writes=[Wb])
                k.op("dve", lambda e: e.tensor_tensor(out=qw[:, 0:Wn], in0=qT[:, 0:Wn], in1=Wb[:, 0:Wn], op=ALU.mult), reads=[qT, Wb], writes=[qw])
                k.op("dve", lambda e: e.tensor_tensor(out=wend[:, 0:n], in0=bcol[:, c0:c0 + n], in1=Ab3[:, 0:n, fc], op=ALU.add), reads=[bcol, Ab], writes=[wend])
                k.op("act", lambda e: e.activation(out=wend[:, 0:n], in_=wend[:, 0:n], func=AF.Exp), reads=[wend], writes=[wend])
                for cc in range(n):
                    k.op(ve(), lambda e, cc=cc: e.tensor_scalar(kw[:, cc * 64:(cc + 1) * 64], ktok[:, cc * 64:(cc + 1) * 64], wend[:, cc:cc + 1], None, op0=ALU.mult), reads=[ktok, wend], writes=[kw.p(cc // 7)])
                for g0 in range(0, n, 7):
                    m = min(7, n - g0)
                    pu = nextps()
                    for i in range(m):
                        cc = g0 + i
                        k.op("pe", lambda e, pu=pu, i=i, cc=cc: e.matmul(pu[0:64, i * 65:(i + 1) * 65], lhsT=kw[:, cc * 64:(cc + 1) * 64], rhs=vaug[:, cc * 65:(cc + 1) * 65], start=True, stop=True),
                             reads=[kw.p(cc // 7), vaug], writes=[pu])
                    k.op("act", lambda e, pu=pu, g0=g0, m=m: e.activation(out=U[:, g0 * 65:(g0 + m) * 65], in_=pu[0:64, 0:m * 65], func=AF.Identity), reads=[pu], writes=[U.p(g0 // 7)])
                order = list(range(n)) if di == 0 else list(range(n - 1, -1, -1))
                first = order[0]
                k.op("dve", lambda e, first=first: e.tensor_copy(CB[:, first * 65:(first + 1) * 65], carry[:]), reads=[carry], writes=[CB.p(first)])
                for oi, cc in enumerate(order):
                    nxt = order[oi + 1] if oi + 1 < n else None
                    dst = CB[:, nxt * 65:(nxt + 1) * 65] if nxt is not None else carry[:]
                    wr = CB.p(nxt) if nxt is not None else carry
                    k.op("dve", lambda e, cc=cc, dst=dst: e.scalar_tensor_tensor(out=dst, in0=CB[:, cc * 65:(cc + 1) * 65], scalar=Wb[:, cc * 64 + fc:cc * 64 + fc + 1],
                                                                                in1=U[:, cc * 65:(cc + 1) * 65], op0=ALU.mult, op1=ALU.add),
                         reads=[CB.p(cc), Wb, U.p(cc // 7)], writes=[wr])
                for cc in range(n):
                    k.op("act", lambda e, cc=cc: e.activation(out=Dm[:, cc * 64:(cc + 1) * 64], in_=Ab[:, cc * 64:(cc + 1) * 64], func=AF.Exp, bias=bcol[:, c0 + cc:c0 + cc + 1], scale=1.0),
                         reads=[Ab, bcol], writes=[Dm.p(cc // 8)])
                for g0 in range(0, n, 8):
                    m = min(8, n - g0)
                    pS = nextps()
                    for i in range(m):
                        cc = g0 + i
                        k.op("pe", lambda e, pS=pS, i=i, cc=cc: e.matmul(pS[0:64, i * 64:(i + 1) * 64], lhsT=kT[:, cc * 64:(cc + 1) * 64], rhs=qT[:, cc * 64:(cc + 1) * 64], start=True, stop=True),
                             reads=[kT, qT], writes=[pS])
                    k.op("dve", lambda e, pS=pS, g0=g0, m=m: e.tensor_tensor(out=SD[:, g0 * 64:(g0 + m) * 64], in0=pS[0:64, 0:m * 64], in1=Dm[:, g0 * 64:(g0 + m) * 64], op=ALU.mult),
                         reads=[pS, Dm.p(g0 // 8)], writes=[SD.p(g0 // 8)])
                    for i in range(m):
                        cc = g0 + i
                        k.op("pool", lambda e, cc=cc: e.tensor_tensor(out=SD[:, cc * 64:(cc + 1) * 64], in0=SD[:, cc * 64:(cc + 1) * 64], in1=tri, op=ALU.mult),
                             reads=[SD.p(g0 // 8), tri2], writes=[SD.p(g0 // 8)])
                    pN = nextps()
                    for i in range(m):
                        cc = g0 + i
                        k.op("pe", lambda e, pN=pN, i=i, cc=cc: e.matmul(pN[0:65, i * 64:(i + 1) * 64], lhsT=vaug[:, cc * 65:(cc + 1) * 65], rhs=SD[:, cc * 64:(cc + 1) * 64], start=True, stop=False),
                             reads=[SD.p(g0 // 8), vaug], writes=[pN])
                        k.op("pe", lambda e, pN=pN, i=i, cc=cc: e.matmul(pN[0:65, i * 64:(i + 1) * 64], lhsT=CB[:, cc * 65:(cc + 1) * 65], rhs=qw[:, cc * 64:(cc + 1) * 64], start=False, stop=True),
                             reads=[qw, CB.p(cc)], writes=[pN])
                    otb = ot[io % 2]; hb_ = hh[io % 2]; io += 1
                    mw = m * 64
                    k.op("act", lambda e, pN=pN, otb=otb, mw=mw: e.activation(out=otb[:, 0:mw], in_=pN[0:65, 0:mw], func=AF.Identity), reads=[pN], writes=[otb])
                    pD = nextps()
                    k.op("pe", lambda e, pD=pD, otb=otb, mw=mw: e.matmul(pD[0:64, 0:mw], lhsT=sel[:, :], rhs=otb[:, 0:mw], start=True, stop=True), reads=[sel, otb], writes=[pD])
                    k.op("act", lambda e, pD=pD, mw=mw: e.activation(out=rr[:, 0:mw], in_=pD[0:64, 0:mw], func=AF.Abs), reads=[pD], writes=[rr])
                    k.op("dve", lambda e, mw=mw: e.tensor_scalar(rr[:, 0:mw], rr[:, 0:mw], 1.0, None, op0=ALU.max), reads=[rr], writes=[rr])
                    k.op("dve", lambda e, mw=mw: e.reciprocal(rr[:, 0:mw], rr[:, 0:mw]), reads=[rr], writes=[rr])
                    k.op("dve", lambda e, otb=otb, hb_=hb_, mw=mw: e.tensor_tensor(out=hb_[:, 0:mw], in0=otb[0:64, 0:mw], in1=rr[:, 0:mw], op=ALU.mult), reads=[otb, rr], writes=[hb_])
                    s0 = 2 + (c0 + g0) * 64
                    k.dma("sp", S2.t[yrow:yrow + 64, s0:s0 + mw], hb_[:, 0:mw], reads=[hb_], writes=[S2.p((di, c0 + g0))])
def f_segments(layer):
    segs = []
    if layer == 0:
        o = 0
        for n in (513, 512, 512, 513):
            segs.append(dict(n=n, which=0, xcol=o, tok=o - 2, ooff=o))
            o += n
        segs.append(dict(n=TCX, which=1, xcol=2052, tok=-1, ooff=2050))
    else:
        for s in range(4):
            segs.append(dict(n=512, which=0, xcol=512 * s, tok=512 * s - 1, ooff=512 * s))
    hc = 0
    tc = 0
    for sg in segs:
        sg["hc"] = hc
        sg["tc"] = tc
        hc += sg["n"] + 2
        tc += sg["n"]
    return segs


def seg_flag_vals(layer, q):
    fl = []
    for sg in f_segments(layer):
        n = sg["n"]
        if sg["which"] == 0:
            base, smax = q * TL + sg["tok"], SEQ
        else:
            base, smax = q * TCX + sg["tok"], CTX
        for col in (0, 1, n, n + 1):
            fl.append(1.0 if 0 <= base + col < smax else 0.0)
    return fl


def g2_off(j, row, which, tok):
    base = (VB * 4 + j) * (Y_ROWS * YW) + row * YW
    if which == 0:
        return base + VJ * TL + (2 + CTX + tok)
    return base + VJ * TCX + (2 + tok)


YB_ROWS = [Y_A, Y_A, Y_B, Y_B, Y_HF, Y_HF, Y_HB, Y_HB, Y_O, Y_O, Y_D, Y_D]


def phase_F(k, layer, W, xsrc, G2, xdst, final, x1d):
    segs = f_segments(layer)
    NH = segs[-1]["hc"] + segs[-1]["n"] + 2
    NT = segs[-1]["tc"] + segs[-1]["n"]
    GT1, SH2, SC2, GT2 = 0, 8, 16, 24
    allfl = [seg_flag_vals(layer, q) for q in range(4)]
    need_flag = [any(allfl[q][i] == 0.0 for q in range(4)) for i in range(4 * len(segs))]
    BW = 258
    with k.phase():
        ones = k.sb([128, 128], F32, "ones")
        k.op("pool", lambda e: e.memset(ones[:], 1.0), writes=[ones])
        vs = k.sb([128, 24], F32, "vs"); cws = k.sb([128, 2 * NFC, 4], F32, "cws"); fls = k.sb([128, 20], F32, "fls")
        for a_, b_ in ((vs, W["vecs"]), (cws, W["cw"]), (fls, W["flg"])):
            k.dma("sp", a_[:], b_[:], writes=[a_])
        mod = k.sb([128, 32, 2], F32, "mod")
        A2 = k.sb([128, 8, 2], F32, "A2")
        h2all = k.sb([128, 8, NH], BF16, "h2all")
        with k.phase():
            bms = k.sb([128, 32], F32, "bms"); cvs = k.sb([128, 8, 2], F32, "cvs")
            k.dma("sp", bms[:], W["bm2"][:], writes=[bms]); k.dma("sp", cvs[:], W["cv"][:], writes=[cvs])
            sc = k.sb([128, 8, 2], F32, "silu_c")
            k.op("act", lambda e: e.activation(out=sc[:], in_=cvs[:], func=AF.Silu), reads=[cvs], writes=[sc])
            pmod = k.ps([128, 512], F32, "pmod")
            wmb = [k.sb([128, 8, 128], F32, "wmb") for i in range(2)]
            wm_v = W["wm2"].t.rearrange("(kc p) c -> p kc c", p=128)
            for j in range(32):
                wb_ = wmb[j % 2]
                k.dma("sp", wb_[:], wm_v[:, :, j * 128:(j + 1) * 128], writes=[wb_])
                for kc in range(8):
                    k.op("pe", lambda e, kc=kc, wb_=wb_: e.matmul(pmod[:, 0:2], lhsT=wb_[:, kc, :], rhs=sc[:, kc, :], start=(kc == 0), stop=(kc == 7)), reads=[wb_, sc], writes=[pmod])
                k.op("dve", lambda e, j=j: e.tensor_scalar(mod[:, j, :], pmod[:, 0:2], bms[:, j:j + 1], None, op0=ALU.add), reads=[pmod, bms], writes=[mod.p(j)])
            k.op("dve", lambda e: e.tensor_scalar(A2[:], mod[:, SC2:SC2 + 8, :], 1.0, None, op0=ALU.add), reads=[mod], writes=[A2])
            for w_ in range(2):
                k.op("dve", lambda e, w_=w_: e.tensor_tensor(out=A2[:, :, w_], in0=A2[:, :, w_], in1=vs[:, 8:16], op=ALU.mult), reads=[A2, vs], writes=[A2])
        with k.phase():
            wob = k.sb([128, 8, D], BF16, "wob")
            wos = [k.sb([128, 8, 128], F32, "wos") for i in range(2)]
            wo_v = W["wo"].t.rearrange("(kc p) c -> p kc c", p=128)
            for mc in range(8):
                s_ = wos[mc % 2]
                k.dma("sp", s_[:], wo_v[:, :, mc * 128:(mc + 1) * 128], writes=[s_])
                k.op("pool", lambda e, s_=s_, mc=mc: e.tensor_copy(wob[:, :, mc * 128:(mc + 1) * 128], s_[:]), reads=[s_], writes=[wob.p(mc)])
            yb = k.sb([128, 12, BW], F32, "yb"); y8 = k.sb([128, 8, BW], F32, "y8"); sq = k.sb([128, 8, BW], F32, "sq")
            yn = k.sb([128, 8, BW], BF16, "yn"); xb = k.sb([128, 8, BW], F32, "xb")
            x1 = [k.sb([128, 8, BW], F32, "x1") for i in range(2)]
            tmp = k.sb([128, BW], F32, "tmp"); rstd = k.sb([128, BW], F32, "rstd")
            pss = k.ps([128, 512], F32, "pss")
            pm = [k.ps([128, 512], F32, "pm") for i in range(3)]
            xT_v = xsrc.t.rearrange("(kc p) t -> p kc t", p=128)
            x1d_v = x1d.t.rearrange("(kc p) t -> p kc t", p=128)
            ibk = 0
            for si, sg_ in enumerate(segs):
                n, which, xcol, tok, hc = sg_["n"], sg_["which"], sg_["xcol"], sg_["tok"], sg_["hc"]
                ncol = n + 2
                blocks = [(0, ncol)] if ncol <= BW else [(0, ncol // 2), (ncol // 2, ncol - ncol // 2)]
                for (c0, cn) in blocks:
                    x1b = x1[ibk % 2]; ibk += 1
                    for ch in range(12):
                        for hf_ in range(2):
                            jh = 2 * (ch % 2) + hf_
                            k.dma_dyn(yb[hf_ * 64:(hf_ + 1) * 64, ch, 0:cn], G2, g2_off(jh, YB_ROWS[ch], which, tok + c0), [[YW, 64], [1, cn]], writes=[yb.p((ch, hf_))])
                    k.dma("sp", xb[:, :, 0:cn], xT_v[:, :, xcol + c0:xcol + c0 + cn], reads=[xsrc], writes=[xb])
                    k.op("pool", lambda e, cn=cn: e.tensor_copy(y8[:, 0:4, 0:cn], yb[:, 0:4, 0:cn]), reads=[yb], writes=[y8.p("ab")])
                    k.op("pool", lambda e, cn=cn: e.tensor_copy(y8[:, 6:8, 0:cn], yb[:, 10:12, 0:cn]), reads=[yb], writes=[y8.p("d")])
                    k.op("dve", lambda e, cn=cn: e.tensor_tensor(out=y8[:, 4:6, 0:cn], in0=yb[:, 4:6, 0:cn], in1=yb[:, 6:8, 0:cn], op=ALU.add), reads=[yb], writes=[y8.p("c")])
                    k.op("act", lambda e, cn=cn: e.activation(out=sq[:, 0:2, 0:cn], in_=yb[:, 8:10, 0:cn], func=AF.Sigmoid), reads=[yb], writes=[sq])
                    k.op("dve", lambda e, cn=cn: e.tensor_tensor(out=y8[:, 4:6, 0:cn], in0=y8[:, 4:6, 0:cn], in1=sq[:, 0:2, 0:cn], op=ALU.mult), reads=[y8.p("c"), sq], writes=[y8.p("c")])
                    k.op("act", lambda e, cn=cn: e.activation(out=sq[:, :, 0:cn], in_=y8[:, :, 0:cn], func=AF.Square), reads=[y8], writes=[sq])
                    for g in range(4):
                        for i in range(2):
                            k.op("pe", lambda e, g=g, i=i, cn=cn: e.matmul(pss[:, 0:cn], lhsT=ones[:], rhs=sq[:, 2 * g + i, 0:cn], start=(i == 0), stop=(i == 1)), reads=[ones, sq], writes=[pss])
                        k.op("act", lambda e, cn=cn: e.activation(out=rstd[:, 0:cn], in_=pss[:, 0:cn], func=AF.Sqrt, scale=1.0 / 256, bias=EPS), reads=[pss], writes=[rstd])
                        k.op("dve", lambda e, cn=cn: e.reciprocal(rstd[:, 0:cn], rstd[:, 0:cn]), reads=[rstd], writes=[rstd])
                        for i in range(2):
                            kc = 2 * g + i
                            k.op("dve", lambda e, kc=kc, cn=cn: e.tensor_tensor(out=tmp[:, 0:cn], in0=y8[:, kc, 0:cn], in1=rstd[:, 0:cn], op=ALU.mult), reads=[y8, rstd], writes=[tmp])
                            k.op("act", lambda e, kc=kc, cn=cn: e.activation(out=yn[:, kc, 0:cn], in_=tmp[:, 0:cn], func=AF.Identity, scale=vs[:, kc:kc + 1]), reads=[tmp, vs], writes=[yn.p(kc)])
                    for mc in range(8):
                        p_ = pm[mc % 3]
                        for kc in range(8):
                            k.op("pe", lambda e, p_=p_, kc=kc, mc=mc, cn=cn: e.matmul(p_[:, 0:cn], lhsT=wob[:, kc, mc * 128:(mc + 1) * 128], rhs=yn[:, kc, 0:cn], start=(kc == 0), stop=(kc == 7)),
                                 reads=[wob.p(mc), yn], writes=[p_])
                        k.op("dve", lambda e, p_=p_, mc=mc, cn=cn, x1b=x1b: e.scalar_tensor_tensor(out=x1b[:, mc, 0:cn], in0=p_[:, 0:cn], scalar=mod[:, GT1 + mc, which:which + 1],
                                                                                            in1=xb[:, mc, 0:cn], op0=ALU.mult, op1=ALU.add), reads=[p_, mod, xb], writes=[x1b.p(mc)])
                    k.dma("sp", x1d_v[:, :, hc + c0:hc + c0 + cn], x1b[:, :, 0:cn], reads=[x1b], writes=[x1d.p((si, c0))])
                    k.op("act", lambda e, cn=cn, x1b=x1b: e.activation(out=sq[:, :, 0:cn], in_=x1b[:, :, 0:cn], func=AF.Square), reads=[x1b], writes=[sq])
                    for kc in range(8):
                        k.op("pe", lambda e, kc=kc, cn=cn: e.matmul(pss[:, 0:cn], lhsT=ones[:], rhs=sq[:, kc, 0:cn], start=(kc == 0), stop=(kc == 7)), reads=[ones, sq], writes=[pss])
                    k.op("act", lambda e, cn=cn: e.activation(out=rstd[:, 0:cn], in_=pss[:, 0:cn], func=AF.Sqrt, scale=1.0 / D, bias=EPS), reads=[pss], writes=[rstd])
                    k.op("dve", lambda e, cn=cn: e.reciprocal(rstd[:, 0:cn], rstd[:, 0:cn]), reads=[rstd], writes=[rstd])
                    for kc in range(8):
                        k.op("dve", lambda e, kc=kc, cn=cn, x1b=x1b: e.tensor_tensor(out=tmp[:, 0:cn], in0=x1b[:, kc, 0:cn], in1=rstd[:, 0:cn], op=ALU.mult), reads=[x1b, rstd], writes=[tmp])
                        k.op("act", lambda e, kc=kc, cn=cn, c0=c0: e.activation(out=h2all[:, kc, hc + c0:hc + c0 + cn], in_=tmp[:, 0:cn], func=AF.Identity, scale=A2[:, kc, which:which + 1], bias=mod[:, SH2 + kc, which:which + 1]),
                             reads=[tmp, A2, mod], writes=[h2all.p((si, c0))])
        with k.phase():
            aT = k.sb([128, NFC, NT], BF16, "aTall")
            with k.phase():
                pu = [k.ps([128, 512], F32, "pu") for i in range(6)]
                wus = [k.sb([128, 8, 256], F32, "wus") for i in range(2)]
                wub = [k.sb([128, 8, 256], BF16, "wub") for i in range(2)]
                ug = [k.sb([128, 516], F32, "ug") for i in range(2)]; uv = [k.sb([128, 516], F32, "uv") for i in range(2)]
                tg = [k.sb([128, 513], F32, "tg") for i in range(2)]; tv = [k.sb([128, 513], F32, "tv") for i in range(2)]
                sgl = [k.sb([128, 513], F32, "sgl") for i in range(2)]
                wu_v = W["wu"].t.rearrange("(kc p) c -> p kc c", p=128)
                it = 0
                ip = 0
                for fc in range(NFC):
                    ws_, wb_ = wus[fc % 2], wub[fc % 2]
                    k.dma("sp", ws_[:, :, 0:128], wu_v[:, :, fc * 128:(fc + 1) * 128], writes=[ws_.p(0)])
                    k.dma("sp", ws_[:, :, 128:256], wu_v[:, :, D_FF + fc * 128:D_FF + (fc + 1) * 128], writes=[ws_.p(1)])
                    k.op("pool", lambda e, ws_=ws_, wb_=wb_: e.tensor_copy(wb_[:], ws_[:]), reads=[ws_], writes=[wb_])
                    for si, sg_ in enumerate(segs):
                        n, hc, tc = sg_["n"], sg_["hc"], sg_["tc"]
                        ncol = n + 2
                        blocks = [(0, ncol)] if ncol <= BW else [(0, ncol // 2), (ncol // 2, ncol - ncol // 2)]
                        ug_, uv_, tg_, tv_, sgl_ = ug[it % 2], uv[it % 2], tg[it % 2], tv[it % 2], sgl[it % 2]
                        it += 1
                        for bi, (c0, cn) in enumerate(blocks):
                            pg_, pv_ = pu[ip % 6], pu[(ip + 1) % 6]
                            ip += 2
                            for kc in range(8):
                                k.op("pe", lambda e, pg_=pg_, kc=kc, wb_=wb_, c0=c0, cn=cn: e.matmul(pg_[:, 0:cn], lhsT=wb_[:, kc, 0:128], rhs=h2all[:, kc, hc + c0:hc + c0 + cn], start=(kc == 0), stop=(kc == 7)), reads=[wb_, h2all], writes=[pg_])
                            for kc in range(8):
                                k.op("pe", lambda e, pv_=pv_, kc=kc, wb_=wb_, c0=c0, cn=cn: e.matmul(pv_[:, 0:cn], lhsT=wb_[:, kc, 128:256], rhs=h2all[:, kc, hc + c0:hc + c0 + cn], start=(kc == 0), stop=(kc == 7)), reads=[wb_, h2all], writes=[pv_])
                            k.op("act", lambda e, pg_=pg_, c0=c0, cn=cn: e.activation(out=ug_[:, c0:c0 + cn], in_=pg_[:, 0:cn], func=AF.Identity), reads=[pg_], writes=[ug_])
                            k.op("act", lambda e, pv_=pv_, c0=c0, cn=cn: e.activation(out=uv_[:, c0:c0 + cn], in_=pv_[:, 0:cn], func=AF.Identity), reads=[pv_], writes=[uv_])
                        for u_ in (ug_, uv_):
                            for fi, col in enumerate((0, 1, ncol - 2, ncol - 1)):
                                if need_flag[4 * si + fi]:
                                    k.op("pool", lambda e, u_=u_, fi=fi, col=col: e.tensor_scalar(u_[:, col:col + 1], u_[:, col:col + 1], fls[:, 4 * si + fi:4 * si + fi + 1], None, op0=ALU.mult), reads=[u_, fls], writes=[u_])
                        for u_, t_, j in ((ug_, tg_, fc), (uv_, tv_, NFC + fc)):
                            k.op("dve", lambda e, u_=u_, t_=t_, j=j: e.tensor_scalar(t_[:, 0:n], u_[:, 0:n], cws[:, j, 0:1], None, op0=ALU.mult), reads=[u_, cws], writes=[t_])
                            k.op("dve", lambda e, u_=u_, t_=t_, j=j: e.scalar_tensor_tensor(out=t_[:, 0:n], in0=u_[:, 1:n + 1], scalar=cws[:, j, 1:2], in1=t_[:, 0:n], op0=ALU.mult, op1=ALU.add), reads=[u_, cws, t_], writes=[t_])
                            k.op("dve", lambda e, u_=u_, t_=t_, j=j: e.scalar_tensor_tensor(out=t_[:, 0:n], in0=u_[:, 2:n + 2], scalar=cws[:, j, 2:3], in1=t_[:, 0:n], op0=ALU.mult, op1=ALU.add), reads=[u_, cws, t_], writes=[t_])
                        k.op("act", lambda e, fc=fc: e.activation(out=sgl_[:, 0:n], in_=tg_[:, 0:n], func=AF.Silu, bias=cws[:, fc, 3:4], scale=1.0), reads=[tg_, cws], writes=[sgl_])
                        k.op("dve", lambda e, fc=fc: e.scalar_tensor_tensor(out=aT[:, fc, tc:tc + n], in0=tv_[:, 0:n], scalar=cws[:, NFC + fc, 3:4], in1=sgl_[:, 0:n], op0=ALU.add, op1=ALU.mult), reads=[tv_, cws, sgl_], writes=[aT.p((fc, si))])
            with k.phase():
                wdb = k.sb([128, NFC, D], BF16, "wdb")
                wds = [k.sb([128, 11, 128], F32, "wds") for i in range(2)]
                wd_v = W["wd"].t.rearrange("(fc p) c -> p fc c", p=128)
                iws = 0
                for mc in range(8):
                    for f0 in (0, 11):
                        ws_ = wds[iws % 2]; iws += 1
                        k.dma("sp", ws_[:], wd_v[:, f0:f0 + 11, mc * 128:(mc + 1) * 128], writes=[ws_])
                        k.op("pool", lambda e, ws_=ws_, mc=mc, f0=f0: e.tensor_copy(wdb[:, f0:f0 + 11, mc * 128:(mc + 1) * 128], ws_[:]), reads=[ws_], writes=[wdb.p((mc, f0))])
                xblk = [k.sb([128, 8, 512], F32, "xblk") for i in range(1)]
                sqm = k.sb([128, 512], F32, "sqm"); rs = k.sb([128, 512], F32, "rs"); tq = k.sb([128, 512], F32, "tq")
                ob = [k.sb([128, 512], F32, "ob") for i in range(2)]
                pm = [k.ps([128, 512], F32, "pm") for i in range(3)]
                pss = k.ps([128, 512], F32, "pss")
                x1d_v = x1d.t.rearrange("(kc p) t -> p kc t", p=128)
                xo_v = xdst.t.rearrange("(kc p) t -> p kc t", p=128)
                ib_ = 0
                io = 0
                for si, sg_ in enumerate(segs):
                    n, which, hc, tc, ooff = sg_["n"], sg_["which"], sg_["hc"], sg_["tc"], sg_["ooff"]
                    nb = (n + 511) // 512
                    bs = (n + nb - 1) // nb
                    for b0 in range(0, n, bs):
                        bn = min(bs, n - b0)
                        xb_ = xblk[0]; ib_ += 1
                        k.dma("sp", xb_[:, :, 0:bn], x1d_v[:, :, hc + 1 + b0:hc + 1 + b0 + bn], reads=[x1d], writes=[xb_])
                        for mc in range(8):
                            p_ = pm[mc % 3]
                            for fc in range(NFC):
                                k.op("pe", lambda e, p_=p_, fc=fc, mc=mc, b0=b0, bn=bn: e.matmul(p_[:, 0:bn], lhsT=wdb[:, fc, mc * 128:(mc + 1) * 128], rhs=aT[:, fc, tc + b0:tc + b0 + bn], start=(fc == 0), stop=(fc == NFC - 1)), reads=[wdb, aT], writes=[p_])
                            k.op("dve", lambda e, p_=p_, mc=mc, bn=bn, xb_=xb_: e.scalar_tensor_tensor(out=xb_[:, mc, 0:bn], in0=p_[:, 0:bn], scalar=mod[:, GT2 + mc, which:which + 1], in1=xb_[:, mc, 0:bn], op0=ALU.mult, op1=ALU.add),
                                 reads=[p_, mod, xb_.p(mc)], writes=[xb_.p(mc)])
                            if final:
                                k.op("act", lambda e, mc=mc, bn=bn, xb_=xb_: e.activation(out=sqm[:, 0:bn], in_=xb_[:, mc, 0:bn], func=AF.Square), reads=[xb_.p(mc)], writes=[sqm])
                                k.op("pe", lambda e, mc=mc, bn=bn: e.matmul(pss[:, 0:bn], lhsT=ones[:], rhs=sqm[:, 0:bn], start=(mc == 0), stop=(mc == 7)), reads=[ones, sqm], writes=[pss])
                        if not final:
                            k.dma("sp", xo_v[:, :, ooff + b0:ooff + b0 + bn], xb_[:, :, 0:bn], reads=[xb_], writes=[xdst.p((si, b0))])
                        else:
                            k.op("act", lambda e, bn=bn: e.activation(out=rs[:, 0:bn], in_=pss[:, 0:bn], func=AF.Sqrt, scale=1.0 / D, bias=EPS), reads=[pss], writes=[rs])
                            k.op("dve", lambda e, bn=bn: e.reciprocal(rs[:, 0:bn], rs[:, 0:bn]), reads=[rs], writes=[rs])
                            for mc in range(8):
                                o_ = ob[io % 2]; io += 1
                                k.op("dve", lambda e, mc=mc, bn=bn, xb_=xb_: e.tensor_tensor(out=tq[:, 0:bn], in0=xb_[:, mc, 0:bn], in1=rs[:, 0:bn], op=ALU.mult), reads=[xb_, rs], writes=[tq])
                                k.op("act", lambda e, mc=mc, o_=o_, bn=bn: e.activation(out=o_[:, 0:bn], in_=tq[:, 0:bn], func=AF.Identity, scale=vs[:, 16 + mc:17 + mc]), reads=[tq, vs], writes=[o_])
                                k.dma("sp", xdst.t[mc * 128:(mc + 1) * 128, ooff + b0:ooff + b0 + bn], o_[:, 0:bn], reads=[o_], writes=[xdst.p((si, b0, mc))], is_output=True)
LAYER_INPUTS = [("wm1", [D, 2048], F32), ("bm1", [128, 16], F32), ("g1", [128, 8], F32), ("wp", [D, NWP], F32), ("gn", [128, 4], F32),
                ("bias", [30, 128, 512], F32), ("sink", [128, 1], F32), ("gb", [64, 4], F32),
                ("wm2", [D, 4096], F32), ("bm2", [128, 32], F32), ("vecs", [128, 24], F32), ("wo", [D, D], F32), ("wu", [D, 2 * D_FF], F32),
                ("cw", [128, 2 * NFC, 4], F32), ("wd", [D_FF, D], F32), ("flg", [128, 20], F32)]
SHARED_INPUTS = [("cv", [128, 8, 2], F32), ("cos", [128, TC], F32), ("sin", [128, TC], F32), ("tri", [64, 2, 64], F32)]
XHW = 2052 + 66
XMW = 2050 + TCX


def build_fused():
    nc = bass.Bass("TRN2", target_bir_lowering=False)
    st = ExitStack()
    with st:
        k = KB(nc, st)
        xT = k.dram("xT", [D, TC], F32, "ExternalInput")
        xTh = k.dram("xTh", [D, XHW], F32, "ExternalInput")
        meta = k.dram("meta", [1, NMETA], I32, "ExternalInput")
        shared = {n: k.dram(n, s, d, "ExternalInput") for n, s, d in SHARED_INPUTS}
        Wl = []
        for l in range(2):
            w = {n: k.dram("%s_%d" % (n, l), s, d, "ExternalInput") for n, s, d in LAYER_INPUTS}
            w.update(shared)
            Wl.append(w)
        out = k.dram("out", [D, TL], F32, "ExternalOutput")
        S1b = [k.dram("S1b%d" % l, [B_ROWS, TC], BF16) for l in range(2)]
        S1c = [k.dram("S1c%d" % l, [C_ROWS, TC], F32) for l in range(2)]
        G1b = [k.dram("G1b%d" % l, [8 * B_ROWS, TC], BF16) for l in range(2)]
        G1c = [k.dram("G1c%d" % l, [8 * C_ROWS, TC], F32) for l in range(2)]
        S2 = k.dram("S2", [Y_ROWS, YW], F32)
        G2 = k.dram("G2", [8 * Y_ROWS, YW], F32)
        xmid = k.dram("xmid", [D, XMW], F32)
        x1d = k.dram("x1d", [D, 2200], F32)
        meta_sb = k.sb([1, NMETA], I32, "meta_sb")
        k.dma("sp", meta_sb[:], meta[:], writes=[meta_sb])
        k.load_coords(meta_sb)
        zt = k.sb([128, 2], F32, "zt")
        k.op("pool", lambda e: e.memset(zt[:], 0.0), writes=[zt])
        for r0 in range(0, Y_ROWS, 128):
            k.dma("sp", S2.t[r0:r0 + 128, 0:2], zt[:], reads=[zt], writes=[S2.p(("pad0", r0))])
            k.dma("sp", S2.t[r0:r0 + 128, TS + 2:TS + 4], zt[:], reads=[zt], writes=[S2.p(("pad1", r0))])
        for l in range(2):
            xsrc, xcol0 = (xT, 0) if l == 0 else (xmid, 1)
            phase_P(k, xsrc, xcol0, Wl[l], S1b[l], S1c[l])
            k.all_gather(S1b[l], G1b[l])
            k.all_gather(S1c[l], G1c[l])
            phase_MA(k, l == 0, Wl[l], G1b[l], S2)
            phase_MC(k, Wl[l], G1c[l], S2)
            k.all_gather(S2, G2)
            if l == 0:
                phase_F(k, 0, Wl[l], xTh, G2, xmid, False, x1d)
            else:
                phase_F(k, 1, Wl[l], xmid, G2, out, True, x1d)
        k.finish()
    return nc


def rope_tables(q):
    t = np.arange(q * TL, (q + 1) * TL)
    row = (t // 64).astype(np.float32)
    col = (t % 64).astype(np.float32)
    freqs = (np.float32(10000.0) ** (-np.arange(16, dtype=np.float32) / 16)).astype(np.float32)
    d = np.arange(64)
    half = d // 32
    i = d % 32
    f = i % 16
    first = i < 16
    pos = np.where(half[:, None] == 0, row[None, :], col[None, :]).astype(np.float32)
    ang = pos * freqs[f][:, None]
    cos = np.cos(ang).astype(np.float32)
    sin = np.sin(ang).astype(np.float32)
    sin = np.where(first[:, None], -sin, sin)
    cos = np.concatenate([cos, np.ones((64, TCX), np.float32)], axis=1)
    sin = np.concatenate([sin, np.zeros((64, TCX), np.float32)], axis=1)
    return np.ascontiguousarray(np.tile(cos, (2, 1))), np.ascontiguousarray(np.tile(sin, (2, 1)))


def chunk_vec(v):
    return np.ascontiguousarray(v.reshape(-1, 128).T)


def b_mask_tiles():
    i = np.arange(128)[:, None]
    jq = np.arange(512)[None, :]
    return np.stack([np.where(np.abs(128 * (r - 1) + i - jq) <= 128, 0.0, NEG).astype(np.float32) for r in range(6)])


def d_bias_tiles(rpb_h):
    i = np.arange(128)[:, None]
    jq = np.arange(512)[None, :]
    out = np.full((3, 8, 128, 512), NEG, np.float32)
    for cls, m in enumerate((0, 5, 15)):
        for o in range(8):
            n = 4 * m - 2 + o
            if not (0 <= n < 64):
                continue
            kr = 2 * n + i // 64
            kc = i % 64
            qr = 8 * m + jq // 64
            qc = jq % 64
            rs = np.clip(qr - 4, 0, 120)
            cs = np.clip(qc - 8, 0, 48)
            valid = (kr >= rs) & (kr < rs + 8) & (kc >= cs) & (kc < cs + 16)
            dr = np.clip(kr - qr + 7, 0, 14)
            dc = np.clip(kc - qc + 15, 0, 30)
            out[cls, o] = np.where(valid, rpb_h[dr, dc], np.float32(NEG))
    return out.reshape(24, 128, 512)


def take_cols(arr, lo, n, smax):
    out = np.zeros((arr.shape[1], n), np.float32)
    a, b = max(lo, 0), min(lo + n, smax)
    out[:, a - lo:b - lo] = arr[a:b].T
    return out


def seg_flags(layer, q):
    fl = seg_flag_vals(layer, q)
    fl += [1.0] * (20 - len(fl))
    return np.tile(np.array(fl, np.float32)[None, :], (128, 1))


_CACHE = {}


def kernel(**inputs):
    inp = {k_: np.ascontiguousarray(np.asarray(v)) for k_, v in inputs.items()}
    if "prog" not in _CACHE:
        _CACHE["prog"] = build_fused()
    nc = _CACHE["prog"]
    bm = b_mask_tiles()
    tri = np.ascontiguousarray(np.stack([np.triu(np.ones((64, 64), np.float32)), np.tril(np.ones((64, 64), np.float32))], axis=1))
    lay = []
    for l in range(2):
        w_mod = inp["w_mod"][l]
        gq, gk = inp["a_q_gain"][l], inp["a_k_gain"][l]
        gn = np.tile(np.stack([gq, gq[SW64], gk, gk[SW64]], axis=1), (2, 1)).astype(np.float32)
        cwf = np.concatenate([inp["conv_w"][l], inp["conv_b"][l][None, :]], axis=0)
        lay.append({
            "wm1": np.ascontiguousarray(w_mod[:, 0:2048]), "bm1": chunk_vec(inp["b_mod"][l][0:2048]), "g1": chunk_vec(inp["g_norm1"][l]),
            "wp": np.ascontiguousarray(inp["w_in"][l][:, P_COLS]), "gn": np.ascontiguousarray(gn),
            "wm2": np.ascontiguousarray(w_mod[:, 2048:6144]), "bm2": chunk_vec(inp["b_mod"][l][2048:6144]),
            "vecs": np.ascontiguousarray(np.concatenate([chunk_vec(inp["g_group"][l]), chunk_vec(inp["g_norm2"][l]), chunk_vec(inp["g_final"])], axis=1)),
            "wo": inp["w_out"][l], "wu": inp["w_up"][l], "cw": np.ascontiguousarray(cwf.T.reshape(2 * NFC, 128, 4).transpose(1, 0, 2)), "wd": inp["w_down"][l],
        })
    maps = []
    for core in range(8):
        b, q = core // 4, core % 4
        j = q
        m = {}
        lo = q * TL
        m["xT"] = np.ascontiguousarray(np.concatenate([take_cols(inp["x"][b], lo, TL, SEQ), take_cols(inp["ctx"][b], q * TCX, TCX, CTX)], axis=1))
        m["xTh"] = np.ascontiguousarray(np.concatenate([take_cols(inp["x"][b], lo - 2, TL + 4, SEQ), take_cols(inp["ctx"][b], q * TCX - 1, TCX + 2, CTX)], axis=1))
        m["meta"] = np.array([[4 * b + j, 2 * b + j // 2, 0, 0, 0, 0, 0, 0]], np.int32)
        m["cv"] = np.ascontiguousarray(np.stack([chunk_vec(inp["c"][b]), chunk_vec(inp["c_ctx"])], axis=2))
        m["cos"], m["sin"] = rope_tables(q)
        m["tri"] = tri
        for l in range(2):
            for n_, v in lay[l].items():
                m["%s_%d" % (n_, l)] = v
            m["bias_%d" % l] = np.ascontiguousarray(np.concatenate([bm, d_bias_tiles(inp["d_rel_bias"][l][j])], axis=0))
            m["sink_%d" % l] = np.full((128, 1), inp["b_sink"][l][j], np.float32)
            gbv = inp["c_gate_bias"][l]
            m["gb_%d" % l] = np.ascontiguousarray(np.tile(np.array([gbv[j], gbv[4 + j], gbv[8 + j], gbv[12 + j]], np.float32)[None, :], (64, 1)))
            m["flg_%d" % l] = seg_flags(l, q)
        maps.append(m)
    res = run_bass_kernel_spmd(nc, maps, core_ids=list(range(8)))
    out = np.zeros((NB, SEQ, D), np.float32)
    for core in range(8):
        b, q = core // 4, core % 4
        out[b, q * TL:(q + 1) * TL, :] = np.asarray(res.results[core]["out"]).T
    return out
```
